# Optimizing a Trainium2 kernel written in Bass

```python
import math
import jax, jax.numpy as jnp
from jax import lax
import numpy as np

D_MODEL = 1024
BATCH = 2
SEQ = 8192
DEPTH = 1

MEM_LEN = 256
EPS = 1e-6
D_FF = 2816
GM_WIDTH = 512
GM_GROUPS = 4
GM_CH = GM_WIDTH // GM_GROUPS
CHUNK = 128
NSA_WIDTH = D_MODEL - GM_WIDTH
HEAD_DIM = 64
NSA_HEADS = NSA_WIDTH // HEAD_DIM
NSA_KV = 2
NSA_REP = NSA_HEADS // NSA_KV
KV_W = NSA_KV * HEAD_DIM
CMP_BLOCK = 32
CMP_STRIDE = 16
CMP_HIDDEN = 256
SEL_BLOCK = 64
N_SELECT = 16
WINDOW = 512
Q_BLOCK = 128
N_BUCKETS = 32
MAX_DISTANCE = 128
XA_HEADS = 4
XA_HEAD_DIM = D_MODEL // XA_HEADS
IN_COLS = 2 * GM_WIDTH + NSA_WIDTH + 6 * KV_W + 3 * NSA_HEADS
NEG = -1e30
FORCE_SCORE = 1e4

kernel_name = 'hybrid_gmlp_nsa_macaron_block'


def rms_norm(x, g):
    xf = x.astype(jnp.float32)
    y = xf * lax.rsqrt(jnp.mean(xf * xf, axis=-1, keepdims=True) + EPS)
    return (y * g.astype(jnp.float32)).astype(x.dtype)


def layer_norm(x, g, b):
    xf = x.astype(jnp.float32)
    mu = jnp.mean(xf, axis=-1, keepdims=True)
    var = jnp.mean(jnp.square(xf - mu), axis=-1, keepdims=True)
    y = (xf - mu) * lax.rsqrt(var + EPS)
    return (y * g.astype(jnp.float32) + b.astype(jnp.float32)).astype(x.dtype)


def swiglu(h, wg, wu, wd):
    return (jax.nn.silu(h @ wg) * (h @ wu)) @ wd


def t5_bucket(dist):
    n = jnp.maximum(dist, 0)
    max_exact = N_BUCKETS // 2
    nf = jnp.maximum(n, 1).astype(jnp.float32)
    large = max_exact + (jnp.log(nf / max_exact) / math.log(MAX_DISTANCE / max_exact)
                         * (N_BUCKETS - max_exact)).astype(jnp.int32)
    large = jnp.minimum(large, N_BUCKETS - 1)
    return jnp.where(n < max_exact, n, large)


def masked_softmax(logits, mask):
    l = jnp.where(mask, logits, NEG)
    m = jnp.max(l, axis=-1, keepdims=True)
    e = jnp.where(mask, jnp.exp(l - m), 0.0)
    return e / jnp.maximum(jnp.sum(e, axis=-1, keepdims=True), 1e-30)


def gmlp_mix(u, v, ln_g, ln_b, ws, bs):
    B, S, _ = u.shape
    u = jax.nn.gelu(u).reshape(B, S // CHUNK, CHUNK, GM_GROUPS, GM_CH)
    v = jax.nn.gelu(v).reshape(B, S // CHUNK, CHUNK, GM_GROUPS, GM_CH)
    v = layer_norm(v, ln_g, ln_b)
    causal = jnp.tril(jnp.ones((CHUNK, CHUNK), dtype=bool))
    w = jnp.where(causal[None], ws, 0.0).astype(v.dtype)
    s = jnp.einsum('gpq,bnqgc->bnpgc', w, v) + bs.T.astype(v.dtype)[None, None, :, :, None]
    return (u * s).reshape(B, S, GM_WIDTH)


def compress(k, pe, w1, b1, w2):
    B, S, G, Dh = k.shape
    units = k.reshape(B, S // CMP_STRIDE, CMP_STRIDE, G, Dh)
    blk = jnp.concatenate([units[:, :-1], units[:, 1:]], axis=2)
    blk = blk + pe[None, None, :, None, :]
    nc = blk.shape[1]
    blk = blk.transpose(0, 1, 3, 2, 4).reshape(B, nc, G, CMP_BLOCK * Dh)
    return jax.nn.gelu(blk @ w1 + b1) @ w2


def nsa_attention(q, kc_raw, vc_raw, ks, vs, kw, vw, gates,
                  ck_pe, ck_w1, ck_b1, ck_w2, cv_pe, cv_w1, cv_b1, cv_w2, rel_bias):
    B, S, G, R, Dh = q.shape
    kc = compress(kc_raw, ck_pe, ck_w1, ck_b1, ck_w2)
    vc = compress(vc_raw, cv_pe, cv_w1, cv_b1, cv_w2)
    nc = kc.shape[1]
    ns = S // SEL_BLOCK
    n_sel = min(N_SELECT, ns)
    nq = S // Q_BLOCK
    scale = HEAD_DIM ** -0.5
    tab = rel_bias.astype(jnp.float32).reshape(N_BUCKETS, G, R)
    cmp_end = jnp.arange(nc) * CMP_STRIDE + CMP_BLOCK - 1
    u_c = CMP_BLOCK // CMP_STRIDE
    u_s = SEL_BLOCK // CMP_STRIDE
    ci = jnp.arange(nc)[:, None]
    sj = jnp.arange(ns)[None, :]
    overlap = jnp.clip(jnp.minimum(ci + u_c, u_s * (sj + 1)) - jnp.maximum(ci, u_s * sj), 0).astype(jnp.float32)
    ks_blk = ks.transpose(0, 2, 1, 3).reshape(B, G, ns, SEL_BLOCK, Dh)
    vs_blk = vs.transpose(0, 2, 1, 3).reshape(B, G, ns, SEL_BLOCK, Dh)
    kw_pad = jnp.pad(kw, ((0, 0), (WINDOW, 0), (0, 0), (0, 0)))
    vw_pad = jnp.pad(vw, ((0, 0), (WINDOW, 0), (0, 0), (0, 0)))
    q_blocks = q.reshape(B, nq, Q_BLOCK, G, R, Dh).transpose(1, 0, 2, 3, 4, 5)
    g_blocks = gates.reshape(B, nq, Q_BLOCK, G, R, 3).transpose(1, 0, 2, 3, 4, 5)
    b_ix = jnp.arange(B)[:, None, None, None]
    g_ix = jnp.arange(G)[None, :, None, None]
    g_ix5 = jnp.arange(G)[None, :, None, None, None]
    blk_start = jnp.arange(ns) * SEL_BLOCK
    j_ix = jnp.arange(ns)

    def one_block(args):
        qb, gb, i = args
        t = i * Q_BLOCK + jnp.arange(Q_BLOCK)
        lc = jnp.einsum('bqgrd,bcgd->bgrqc', qb, kc).astype(jnp.float32) * scale
        lc = lc + tab[t5_bucket(t[:, None] - cmp_end[None, :])].transpose(2, 3, 0, 1)
        pc = masked_softmax(lc, cmp_end[None, :] <= t[:, None])
        oc = jnp.einsum('bgrqc,bcgd->bqgrd', pc.astype(vc.dtype), vc)
        imp = jnp.einsum('bgrqc,cj->bgqj', pc, overlap)
        cur = t // SEL_BLOCK
        causal = blk_start[None, :] <= t[:, None]
        forced = (j_ix[None, :] == 0) | (j_ix[None, :] == cur[:, None]) | (j_ix[None, :] == cur[:, None] - 1)
        score = jnp.where(forced, FORCE_SCORE, jnp.where(causal, imp, -1.0))
        top_score, top_idx = lax.top_k(score, n_sel)
        blk_ok = top_score >= 0.0
        ksel = ks_blk[b_ix, g_ix, top_idx]
        vsel = vs_blk[b_ix, g_ix, top_idx]
        pos = top_idx[..., None] * SEL_BLOCK + jnp.arange(SEL_BLOCK)
        dist = t[None, None, :, None, None] - pos
        ls = jnp.einsum('bqgrd,bgqnkd->bgrqnk', qb, ksel).astype(jnp.float32) * scale
        ls = ls + tab[t5_bucket(dist), g_ix5].transpose(0, 1, 5, 2, 3, 4)
        ms = (blk_ok[..., None] & (dist >= 0))[:, :, None]
        L = n_sel * SEL_BLOCK
        ps = masked_softmax(ls.reshape(B, G, R, Q_BLOCK, L), ms.reshape(B, G, 1, Q_BLOCK, L))
        osl = jnp.einsum('bgrql,bgqld->bqgrd', ps.astype(vsel.dtype), vsel.reshape(B, G, Q_BLOCK, L, Dh))
        start = i * Q_BLOCK
        kwb = lax.dynamic_slice_in_dim(kw_pad, start, Q_BLOCK + WINDOW, axis=1)
        vwb = lax.dynamic_slice_in_dim(vw_pad, start, Q_BLOCK + WINDOW, axis=1)
        pos_w = start - WINDOW + jnp.arange(Q_BLOCK + WINDOW)
        dw = t[:, None] - pos_w[None, :]
        mw = (pos_w[None, :] >= 0) & (dw >= 0) & (dw < WINDOW)
        lw = jnp.einsum('bqgrd,bkgd->bgrqk', qb, kwb).astype(jnp.float32) * scale
        lw = lw + tab[t5_bucket(dw)].transpose(2, 3, 0, 1)
        pw = masked_softmax(lw, mw)
        ow = jnp.einsum('bgrqk,bkgd->bqgrd', pw.astype(vwb.dtype), vwb)
        g = jax.nn.sigmoid(gb.astype(jnp.float32))
        o = g[..., 0:1] * oc + g[..., 1:2] * osl + g[..., 2:3] * ow
        return o.reshape(B, Q_BLOCK, NSA_WIDTH).astype(qb.dtype)

    out = lax.map(one_block, (q_blocks, g_blocks, jnp.arange(nq)))
    return out.transpose(1, 0, 2, 3).reshape(B, S, NSA_WIDTH)


def cross_attention(h, mem, g_mem, wq, wkv, wo):
    B, S, _ = h.shape
    M = mem.shape[1]
    q = (h @ wq).reshape(B, S, XA_HEADS, XA_HEAD_DIM)
    k, v = jnp.split(rms_norm(mem, g_mem) @ wkv, 2, axis=-1)
    k = k.reshape(B, M, XA_HEADS, XA_HEAD_DIM)
    v = v.reshape(B, M, XA_HEADS, XA_HEAD_DIM)
    logits = jnp.einsum('bshd,bmhd->bhsm', q, k).astype(jnp.float32) * (XA_HEAD_DIM ** -0.5)
    p = jax.nn.softmax(logits, axis=-1)
    o = jnp.einsum('bhsm,bmhd->bshd', p.astype(v.dtype), v).reshape(B, S, D_MODEL)
    return o @ wo


def setup_inputs(seed: int = 0) -> dict:
    key = jax.random.key(seed)
    ks = iter(jax.random.split(key, 48))
    L = DEPTH

    def nrm(shape, scale):
        return jax.random.normal(next(ks), shape, jnp.float32) * scale

    def gain(shape):
        return 1.0 + nrm(shape, 0.05)

    return {
        'x': nrm((BATCH, SEQ, D_MODEL), 1.0),
        'mem': nrm((BATCH, MEM_LEN, D_MODEL), 1.0),
        'ffn1_pre': gain((L, D_MODEL)),
        'ffn1_post': gain((L, D_MODEL)),
        'ffn1_wg': nrm((L, D_MODEL, D_FF), D_MODEL ** -0.5),
        'ffn1_wu': nrm((L, D_MODEL, D_FF), D_MODEL ** -0.5),
        'ffn1_wd': nrm((L, D_FF, D_MODEL), D_FF ** -0.5),
        'mix_pre': gain((L, D_MODEL)),
        'mix_post': gain((L, D_MODEL)),
        'w_in': nrm((L, D_MODEL, IN_COLS), D_MODEL ** -0.5),
        'gm_ln_g': gain((L, GM_GROUPS, GM_CH)),
        'gm_ln_b': nrm((L, GM_GROUPS, GM_CH), 0.02),
        'gm_ws': nrm((L, GM_GROUPS, CHUNK, CHUNK), CHUNK ** -0.5),
        'gm_bs': 1.0 + nrm((L, GM_GROUPS, CHUNK), 0.1),
        'ck_pe': nrm((L, CMP_BLOCK, HEAD_DIM), 0.1),
        'ck_w1': nrm((L, CMP_BLOCK * HEAD_DIM, CMP_HIDDEN), (CMP_BLOCK * HEAD_DIM) ** -0.5),
        'ck_b1': nrm((L, CMP_HIDDEN), 0.02),
        'ck_w2': nrm((L, CMP_HIDDEN, HEAD_DIM), CMP_HIDDEN ** -0.5),
        'cv_pe': nrm((L, CMP_BLOCK, HEAD_DIM), 0.1),
        'cv_w1': nrm((L, CMP_BLOCK * HEAD_DIM, CMP_HIDDEN), (CMP_BLOCK * HEAD_DIM) ** -0.5),
        'cv_b1': nrm((L, CMP_HIDDEN), 0.02),
        'cv_w2': nrm((L, CMP_HIDDEN, HEAD_DIM), CMP_HIDDEN ** -0.5),
        'rel_bias': nrm((N_BUCKETS, NSA_HEADS), 0.5),
        'out_gain_a': gain((L, GM_WIDTH)),
        'out_gain_b': gain((L, NSA_WIDTH)),
        'w_out': nrm((L, D_MODEL, D_MODEL), D_MODEL ** -0.5),
        'xa_pre': gain((L, D_MODEL)),
        'xa_post': gain((L, D_MODEL)),
        'mem_norm': gain((L, D_MODEL)),
        'xa_wq': nrm((L, D_MODEL, D_MODEL), D_MODEL ** -0.5),
        'xa_wkv': nrm((L, D_MODEL, 2 * D_MODEL), D_MODEL ** -0.5),
        'xa_wo': nrm((L, D_MODEL, D_MODEL), D_MODEL ** -0.5),
        'ffn2_pre': gain((L, D_MODEL)),
        'ffn2_post': gain((L, D_MODEL)),
        'ffn2_wg': nrm((L, D_MODEL, D_FF), D_MODEL ** -0.5),
        'ffn2_wu': nrm((L, D_MODEL, D_FF), D_MODEL ** -0.5),
        'ffn2_wd': nrm((L, D_FF, D_MODEL), D_FF ** -0.5),
    }


def reference(x, mem, ffn1_pre, ffn1_post, ffn1_wg, ffn1_wu, ffn1_wd,
              mix_pre, mix_post, w_in, gm_ln_g, gm_ln_b, gm_ws, gm_bs,
              ck_pe, ck_w1, ck_b1, ck_w2, cv_pe, cv_w1, cv_b1, cv_w2, rel_bias,
              out_gain_a, out_gain_b, w_out, xa_pre, xa_post, mem_norm, xa_wq, xa_wkv, xa_wo,
              ffn2_pre, ffn2_post, ffn2_wg, ffn2_wu, ffn2_wd):
    B, S, _ = x.shape
    widths = [GM_WIDTH, GM_WIDTH, NSA_WIDTH] + [KV_W] * 6 + [3 * NSA_HEADS]
    offsets = np.cumsum(widths)[:-1].tolist()
    for l in range(DEPTH):
        h = rms_norm(x, ffn1_pre[l])
        x = x + 0.5 * rms_norm(swiglu(h, ffn1_wg[l], ffn1_wu[l], ffn1_wd[l]), ffn1_post[l])
        h = rms_norm(x, mix_pre[l])
        z = h @ w_in[l]
        u, v, q, kc, vc, ksl, vsl, kwn, vwn, gt = jnp.split(z, offsets, axis=-1)
        y_a = gmlp_mix(u, v, gm_ln_g[l], gm_ln_b[l], gm_ws[l], gm_bs[l])
        kv_shape = (B, S, NSA_KV, HEAD_DIM)
        y_b = nsa_attention(q.reshape(B, S, NSA_KV, NSA_REP, HEAD_DIM),
                            kc.reshape(kv_shape), vc.reshape(kv_shape),
                            ksl.reshape(kv_shape), vsl.reshape(kv_shape),
                            kwn.reshape(kv_shape), vwn.reshape(kv_shape), gt,
                            ck_pe[l], ck_w1[l], ck_b1[l], ck_w2[l],
                            cv_pe[l], cv_w1[l], cv_b1[l], cv_w2[l], rel_bias)
        y = jnp.concatenate([rms_norm(y_a, out_gain_a[l]), rms_norm(y_b, out_gain_b[l])], axis=-1) @ w_out[l]
        x = x + rms_norm(y, mix_post[l])
        h = rms_norm(x, xa_pre[l])
        x = x + rms_norm(cross_attention(h, mem, mem_norm[l], xa_wq[l], xa_wkv[l], xa_wo[l]), xa_post[l])
        h = rms_norm(x, ffn2_pre[l])
        x = x + 0.5 * rms_norm(swiglu(h, ffn2_wg[l], ffn2_wu[l], ffn2_wd[l]), ffn2_post[l])
    return x
```

```python
import math
from contextlib import ExitStack

import numpy as np
import concourse.bass as bass
import concourse.mybir as mybir
from concourse.bass_utils import run_bass_kernel_spmd

F32 = mybir.dt.float32
BF16 = mybir.dt.bfloat16
AF = mybir.ActivationFunctionType
ALU = mybir.AluOpType
AX = mybir.AxisListType
ENG = ('pe', 'act', 'dve', 'pool', 'sp')

D = 1024
DFF = 2816
NJ = DFF // 128
SEQ = 8192
NT = 64
NOWN = 16
SBT = 8
SBN = SBT * 128
EPS = 1e-6
NEG = -30000.0
BIGNEG = 32768.0
SCALE = 0.125

C_U, C_V, C_Q, C_KCR, C_VCR, C_KSL, C_KWN, C_VSL, C_VWN, C_GT = 0, 512, 1024, 1536, 1664, 1792, 1920, 2048, 2176, 2304
G_F1PRE, G_F1POST, G_MIXPRE, G_MIXPOST, G_XAPRE, G_XAPOST, G_F2PRE, G_F2POST = [8 * i for i in range(8)]
G_OGA, G_CKB1, G_CVB1, NGCOL = 64, 68, 70, 72
R_LNG, R_LNB, R_OGB, R_MEMN, R_BS, NROW = 0, 512, 1024, 1536, 2560, 3072


class Buf:
    __slots__ = ('name', 'w', 'r', 'dsem', 'dcnt')

    def __init__(self, name):
        self.name = name
        self.w = None
        self.r = {}
        self.dsem = None
        self.dcnt = 0


class Prog:
    def __init__(self, nc, es):
        self.nc = nc
        self.es = es
        self.q = {k: [] for k in ENG}
        self.sem = {k: es.enter_context(nc.semaphore('s_' + k)) for k in ENG}
        self.cnt = {k: 0 for k in ENG}
        self.known = {k: {} for k in ENG}
        self.dbufs = []
        self.out_waits = []

    def _deps(self, reads, writes, skip_waw=()):
        d = {}
        for b in reads:
            if b.w is not None and d.get(b.w[0], 0) < b.w[1]:
                d[b.w[0]] = b.w[1]
        for b in writes:
            if b.w is not None and b not in skip_waw and d.get(b.w[0], 0) < b.w[1]:
                d[b.w[0]] = b.w[1]
            for sem, v in b.r.items():
                if d.get(sem, 0) < v:
                    d[sem] = v
        return d

    def _waits(self, eng, d):
        waits = []
        kn = self.known[eng]
        for sem, v in d.items():
            if eng == 'pe' and sem is self.sem['pe']:
                continue
            if kn.get(sem, 0) < v:
                waits.append((sem, v))
                kn[sem] = v
        return waits

    def op(self, eng, fn, reads=(), writes=()):
        waits = self._waits(eng, self._deps(reads, writes))
        self.cnt[eng] += 1
        n = self.cnt[eng]
        mysem = self.sem[eng]

        def thunk(e):
            for sem, v in waits:
                e.wait_ge(sem, v)
            fn(e).then_inc(mysem, 1)
        self.q[eng].append(thunk)
        for b in writes:
            b.w = (mysem, n)
            b.r = {}
        for b in reads:
            b.r[mysem] = n

    def dma(self, eng, out_ap, in_ap, reads=(), writes=(), parallel=True, is_output=False, sem_owner=None):
        wb = sem_owner if sem_owner is not None else writes[0]
        if wb.dsem is None:
            wb.dsem = self.es.enter_context(self.nc.semaphore('d_' + wb.name))
            self.dbufs.append(wb)
        skip = tuple(b for b in writes if parallel and b.w is not None and (b.w[0] is b.dsem or sem_owner is not None))
        waits = self._waits(eng, self._deps(reads, writes, skip_waw=skip))
        wb.dcnt += 16
        v = wb.dcnt
        sem = wb.dsem

        def thunk(e):
            for s_, v_ in waits:
                e.wait_ge(s_, v_)
            e.dma_start(out=out_ap, in_=in_ap).then_inc(sem, 16)
        self.q[eng].append(thunk)
        for b in writes:
            b.w = (sem, v)
            b.r = {}
        for b in reads:
            b.r[sem] = v
        if is_output:
            self.out_waits.append((sem, v))

    def barrier(self):
        d = {self.sem[k]: self.cnt[k] for k in ENG if self.cnt[k] > 0}
        for b in self.dbufs:
            d[b.dsem] = b.dcnt
        for k in ENG:
            waits = self._waits(k, dict(d))
            if waits:
                self.q[k].append(lambda e, waits=waits: [e.wait_ge(s_, v_) for s_, v_ in waits])

    def finish(self):
        d = {}
        for sem, v in self.out_waits:
            if d.get(sem, 0) < v:
                d[sem] = v
        waits = list(d.items())
        self.q['sp'].append(lambda e: [e.wait_ge(s_, v_) for s_, v_ in waits])

    def emit(self):
        nc = self.nc
        q = self.q
        with nc.Block() as block:
            @block.tensor
            def _(e):
                for f in q['pe']:
                    f(e)

            @block.scalar
            def _(e):
                for f in q['act']:
                    f(e)

            @block.vector
            def _(e):
                for f in q['dve']:
                    f(e)

            @block.gpsimd
            def _(e):
                for f in q['pool']:
                    f(e)

            @block.sync
            def _(e):
                for f in q['sp']:
                    f(e)
        self.q = {k: [] for k in ENG}


def build(stage=99, debug=False):
    nc = bass.Bass("TRN2", target_bir_lowering=False)

    def din(name, shape):
        return nc.dram_tensor(name, list(shape), F32, kind="ExternalInput")

    x_d = din("x_ctx", [SEQ, D])
    mem_d = din("mem", [256, D])
    wg_d = [din("wg1", [D, DFF]), din("wg2", [D, DFF])]
    wu_d = [din("wu1", [D, DFF]), din("wu2", [D, DFF])]
    wd_d = [din("wd1", [DFF, D]), din("wd2", [DFF, D])]
    win_d = din("w_in", [D, 2328])
    wout_d = din("w_out", [D, D])
    ws_d = din("gm_ws", [4, 128, 128])
    cw1_d = [din("ck_w1", [2048, 256]), din("cv_w1", [2048, 256])]
    cw2_d = [din("ck_w2", [256, 64]), din("cv_w2", [256, 64])]
    xwq_d = din("xa_wq", [D, D])
    xwkv_d = din("xa_wkv", [D, 2 * D])
    xwo_d = din("xa_wo", [D, D])
    gcol_d = din("gcol", [128, NGCOL])
    peT_d = din("peT", [64, 64])
    rowv_d = din("rowv", [1, NROW])
    tab_d = din("rel_bias", [32, 8])
    ohd_d = din("ohd", [33, 768])
    smask_d = din("smask", [128, NOWN, 2, 128])
    kb_d = din("kb", [128, NT])
    kbc_d = din("kbc", [128, 4])
    kbn_d = din("kbn", [16, NOWN])
    ovf_d = din("ovf", [128, 4, 128])
    ovn_d = din("ovn", [16, NOWN, 128])
    y_d = nc.dram_tensor("y_own", [NOWN * 128, D], F32, kind="ExternalOutput")

    skind = "ExternalOutput" if debug else "Internal"
    kt_d = [nc.dram_tensor("sc_kt%d" % s, [128, SEQ], BF16, kind=skind) for s in range(4)]
    vt_d = nc.dram_tensor("sc_vt", [SEQ, 256], BF16, kind=skind)
    x1_d = nc.dram_tensor("sc_x1", [128, 8, NOWN * 128], F32, kind=skind)
    fv_d = nc.dram_tensor("sc_fv", [8, 768], F32, kind="Internal")

    with ExitStack() as es:
        P = Prog(nc, es)

        def sbuf(st, name, shape, dt):
            return st.enter_context(nc.sbuf_tensor("sb_" + name, list(shape), dt))

        ps = [es.enter_context(nc.psum_tensor("ps%d" % i, [128, 512], F32)) for i in range(8)]
        psb = [Buf("ps%d" % i) for i in range(8)]

        ones_bf = sbuf(es, "ones_bf", [128, 128], BF16)
        ones_f = sbuf(es, "ones_f", [128, 128], F32)
        ident = sbuf(es, "ident", [128, 128], F32)
        antid = sbuf(es, "antid", [128, 128], F32)
        gcol = sbuf(es, "gcol", [128, NGCOL], F32)
        ghalf = sbuf(es, "ghalf", [128, 16], F32)
        zcol = sbuf(es, "zcol", [128, 1], F32)
        B_const = Buf("const")
        B_ghalf = Buf("ghalf")
        P.op('pool', lambda e: e.memset(ones_f[:], 1.0), writes=[B_const])
        P.op('pool', lambda e: e.memset(ones_bf[:], 1.0), writes=[B_const])
        P.op('pool', lambda e: e.memset(zcol[:], 0.0), writes=[B_const])
        P.op('pool', lambda e: e.affine_select(out=ident[:], in_=ones_f[:], pattern=[[-1, 128]], compare_op=ALU.is_equal,
                                               fill=0.0, base=0, channel_multiplier=1), reads=[B_const], writes=[B_const])
        P.op('pool', lambda e: e.affine_select(out=antid[:], in_=ones_f[:], pattern=[[1, 128]], compare_op=ALU.is_equal,
                                               fill=0.0, base=-127, channel_multiplier=1), reads=[B_const], writes=[B_const])
        B_gcol = Buf("gcol")
        P.dma('sp', gcol[:], gcol_d[:, :], writes=[B_gcol])
        P.op('dve', lambda e: e.tensor_scalar(out=ghalf[:, 0:8], in0=gcol[:, G_F1POST:G_F1POST + 8], scalar1=0.5, scalar2=None, op0=ALU.mult),
             reads=[B_gcol], writes=[B_ghalf])
        P.op('dve', lambda e: e.tensor_scalar(out=ghalf[:, 8:16], in0=gcol[:, G_F2POST:G_F2POST + 8], scalar1=0.5, scalar2=None, op0=ALU.mult),
             reads=[B_gcol], writes=[B_ghalf])

        def bsel(b, tb):
            return b[tb] if isinstance(b, (list, tuple)) else b

        def rstd_from_stat(st, stat_ps, stat_b, rs_t, rs_b, n, width, rows=128):
            P.op('act', lambda e: e.activation(out=rs_t[:rows, :width], in_=stat_ps[:rows, :width], func=AF.Ln, scale=1.0 / n, bias=EPS),
                 reads=[stat_b], writes=[rs_b])
            P.op('act', lambda e: e.activation(out=rs_t[:rows, :width], in_=rs_t[:rows, :width], func=AF.Exp, scale=-0.5), reads=[rs_b], writes=[rs_b])

        def rms_fm(xT, xb, gofs, hT, hb, ntok, sq, sqb, rs, rsb, stat_i=6, t_start=0):
            for t0 in range(t_start, t_start + ntok, 512):
                w = min(512, t_start + ntok - t0)
                tb = t0 // 512
                xb_, hb_, rs_, rsb_ = bsel(xb, tb), bsel(hb, tb), bsel(rs, tb), bsel(rsb, tb)
                for c in range(8):
                    P.op('act', lambda e, c=c, t0=t0, w=w: e.activation(out=sq[:, c, :w], in_=xT[:, c, t0:t0 + w], func=AF.Square),
                         reads=[xb_], writes=[sqb[c]])
                    P.op('pe', lambda e, c=c, w=w: e.matmul(ps[stat_i][:, :w], lhsT=ones_bf[:], rhs=sq[:, c, :w], start=(c == 0), stop=(c == 7)),
                         reads=[sqb[c], B_const], writes=[psb[stat_i]])
                rstd_from_stat(None, ps[stat_i], psb[stat_i], rs_, rsb_, float(D), w)
                for c in range(8):
                    P.op('dve', lambda e, c=c, t0=t0, w=w, rs_=rs_: e.scalar_tensor_tensor(out=hT[:, c, t0:t0 + w], in0=xT[:, c, t0:t0 + w],
                                                                                         scalar=gcol[:, gofs + c:gofs + c + 1], in1=rs_[:, :w],
                                                                                         op0=ALU.mult, op1=ALU.mult),
                         reads=[xb_, rsb_, B_gcol], writes=[hb_])

        class WStream:
            def __init__(self, st, name, kc, nbuf):
                self.kc = kc
                self.t = [sbuf(st, "%s%d" % (name, i), [128, kc, 128], BF16) for i in range(nbuf)]
                self.b = [Buf("%s%d" % (name, i)) for i in range(nbuf)]
                self.n = 0

            def load(self, w_dram, c):
                i = self.n % len(self.t)
                self.n += 1
                src = w_dram[:, c * 128:(c + 1) * 128].rearrange("(kc p) m -> p kc m", p=128)
                P.dma('pool', self.t[i][:], src, writes=[self.b[i]], parallel=False)
                return self.t[i], self.b[i]

        def lin_post(st, src, srcb, KC, w_dram, wstream, xT, xb, ntok, gain_ap_fn, yT, yb_, sq, sqb, rs, rsb, after_block=None):
            nb = (ntok + 511) // 512
            pend_stat = None
            nxt = wstream.load(w_dram, 0)
            for c in range(8):
                wt, wb = nxt
                if c + 1 < 8:
                    nxt = wstream.load(w_dram, c + 1)
                for tb in range(nb):
                    t0 = tb * 512
                    w = min(512, ntok - t0)
                    yi = 4 + (c * nb + tb) % 2
                    for k in range(KC):
                        P.op('pe', lambda e, k=k, t0=t0, w=w, wt=wt, yi=yi: e.matmul(ps[yi][:, :w], lhsT=wt[:, k, :], rhs=src[:, k, t0:t0 + w],
                                                                                   start=(k == 0), stop=(k == KC - 1)),
                             reads=[wb, bsel(srcb, tb)], writes=[psb[yi]])
                    P.op('act', lambda e, c=c, t0=t0, w=w, yi=yi: e.activation(out=yT[:, c, t0:t0 + w], in_=ps[yi][:, :w], func=AF.Copy),
                         reads=[psb[yi]], writes=[bsel(yb_, tb)])
                    P.op('act', lambda e, c=c, w=w, yi=yi, tb=tb: e.activation(out=sq[:, (c * nb + tb) % 8, :w], in_=ps[yi][:, :w], func=AF.Square),
                         reads=[psb[yi]], writes=[sqb[(c * nb + tb) % 8]])
                    if pend_stat is not None:
                        pend_stat()
                    pend_stat = (lambda c=c, w=w, tb=tb: P.op(
                        'pe', lambda e: e.matmul(ps[6 + tb][:, :w], lhsT=ones_bf[:], rhs=sq[:, (c * nb + tb) % 8, :w], start=(c == 0), stop=(c == 7)),
                        reads=[sqb[(c * nb + tb) % 8], B_const], writes=[psb[6 + tb]]))
            if pend_stat is not None:
                pend_stat()
            for tb in range(nb):
                t0 = tb * 512
                w = min(512, ntok - t0)
                rs_, rsb_, ybb, xbb = bsel(rs, tb), bsel(rsb, tb), bsel(yb_, tb), bsel(xb, tb)
                rstd_from_stat(None, ps[6 + tb], psb[6 + tb], rs_, rsb_, float(D), w)
                for c in range(8):
                    P.op('dve', lambda e, c=c, t0=t0, w=w, rs_=rs_: e.scalar_tensor_tensor(out=yT[:, c, t0:t0 + w], in0=yT[:, c, t0:t0 + w],
                                                                                         scalar=gain_ap_fn(c), in1=rs_[:, :w], op0=ALU.mult, op1=ALU.mult),
                         reads=[ybb, rsb_, B_gcol, B_ghalf], writes=[ybb])
                    P.op('dve', lambda e, c=c, t0=t0, w=w: e.tensor_tensor(out=xT[:, c, t0:t0 + w], in0=xT[:, c, t0:t0 + w], in1=yT[:, c, t0:t0 + w], op=ALU.add),
                         reads=[ybb, xbb], writes=[xbb])
                if after_block is not None:
                    after_block(tb)

        def ffn(st, li, xT, xb, hT, hb, actT, actb, yT, yb_, sq, sqb, rs, rsb, sg, sgb, wgs, wus, wds, gpre, ghalf_ofs, after_block=None):
            rms_fm(xT, xb, gpre, hT, hb, SBN, sq, sqb, rs, rsb)
            nxt = (wgs.load(wg_d[li], 0), wus.load(wu_d[li], 0))
            for j in range(NJ):
                (gt_, gb_), (ut_, ub_) = nxt
                if j + 1 < NJ:
                    nxt = (wgs.load(wg_d[li], j + 1), wus.load(wu_d[li], j + 1))
                for tb in range(2):
                    t0 = tb * 512
                    gi = (j * 2 + tb) % 2
                    ui = 2 + gi
                    for k in range(8):
                        P.op('pe', lambda e, k=k, t0=t0, gt_=gt_, gi=gi: e.matmul(ps[gi][:], lhsT=gt_[:, k, :], rhs=hT[:, k, t0:t0 + 512],
                                                                                start=(k == 0), stop=(k == 7)),
                             reads=[gb_, bsel(hb, tb)], writes=[psb[gi]])
                    for k in range(8):
                        P.op('pe', lambda e, k=k, t0=t0, ut_=ut_, ui=ui: e.matmul(ps[ui][:], lhsT=ut_[:, k, :], rhs=hT[:, k, t0:t0 + 512],
                                                                                start=(k == 0), stop=(k == 7)),
                             reads=[ub_, bsel(hb, tb)], writes=[psb[ui]])
                    P.op('act', lambda e, gi=gi: e.activation(out=sg[gi][:], in_=ps[gi][:], func=AF.Silu), reads=[psb[gi]], writes=[sgb[gi]])
                    P.op('dve', lambda e, gi=gi, ui=ui, j=j, t0=t0: e.tensor_tensor(out=actT[:, j, t0:t0 + 512], in0=sg[gi][:], in1=ps[ui][:], op=ALU.mult),
                         reads=[sgb[gi], psb[ui]], writes=[bsel(actb, tb)])
            lin_post(st, actT, actb, NJ, wd_d[li], wds, xT, xb, SBN, lambda c: ghalf[:, ghalf_ofs + c:ghalf_ofs + c + 1], yT, yb_, sq, sqb, rs, rsb,
                     after_block=after_block)

        n_sb = NT // SBT if stage >= 2 else 1
        with ExitStack() as ph:
            xT = sbuf(ph, "p1_xT", [128, 8, SBN], F32)
            hT = sbuf(ph, "p1_hT", [128, 8, SBN], BF16)
            actT = sbuf(ph, "p1_actT", [128, NJ, SBN], BF16)
            yT = sbuf(ph, "p1_yT", [128, 8, SBN], F32)
            sq = sbuf(ph, "p1_sq", [128, 8, 512], BF16)
            rs = sbuf(ph, "p1_rs", [128, 512], F32)
            sg = [sbuf(ph, "p1_sg%d" % i, [128, 512], F32) for i in range(2)]
            xin = [sbuf(ph, "p1_xin%d" % i, [128, D], F32) for i in range(4)]
            wkf = sbuf(ph, "p1_wkf", [128, 8, 512], BF16)
            wvt = sbuf(ph, "p1_wvt", [128, 8, 256], BF16)
            kst = sbuf(ph, "p1_kst", [128, 4, SBN], BF16)
            vst = sbuf(ph, "p1_vst", [128, SBT, 256], BF16)
            xb, hb, actb, yb_, rsb = [[Buf(n + "0"), Buf(n + "1")] for n in ("xT", "hT", "actT", "yT", "rs")]
            rs = [rs, sbuf(ph, "p1_rs1", [128, 512], F32)]
            sqb = [Buf("sq%d" % i) for i in range(8)]
            sgb = [Buf("sg0"), Buf("sg1")]
            xinb = [Buf("xin%d" % i) for i in range(4)]
            B_wk = Buf("wkf")
            kstb = [[Buf("kst%d%d" % (a_, b_)) for b_ in range(4)] for a_ in range(2)]
            vstb = [Buf("vst0"), Buf("vst1")]
            xstb = [Buf("xst0"), Buf("xst1")]
            ktdb = [Buf("ktd%d" % s) for s in range(4)]
            vtdb, x1db = Buf("vtd"), Buf("x1d")
            wgs = WStream(ph, "p1_wg", 8, 3)
            wus = WStream(ph, "p1_wu", 8, 3)
            wds = WStream(ph, "p1_wd", NJ, 2)
            P.dma('pool', wkf[:], win_d[:, C_KCR:C_KCR + 512].rearrange("(kc p) m -> p kc m", p=128), writes=[B_wk])
            P.dma('pool', wvt[:], win_d[:, C_VSL:C_VSL + 256].rearrange("(kc p) m -> p kc m", p=128), writes=[B_wk])

            for sbi in range(n_sb):
                for t in range(SBT):
                    gt = sbi * SBT + t
                    xi = gt % 4
                    P.dma('sp', xin[xi][:], x_d[gt * 128:(gt + 1) * 128, :], writes=[xinb[xi]], parallel=False)
                    for half in range(2):
                        for cc in range(4):
                            c = half * 4 + cc
                            P.op('pe', lambda e, xi=xi, c=c, cc=cc: e.transpose(ps[7][:, cc * 128:(cc + 1) * 128], xin[xi][:, c * 128:(c + 1) * 128], ident[:]),
                                 reads=[xinb[xi], B_const], writes=[psb[7]])
                        P.op('dve', lambda e, half=half, t=t: e.tensor_copy(out=xT[:, half * 4:half * 4 + 4, t * 128:(t + 1) * 128],
                                                                          in_=ps[7][:].rearrange("p (c q) -> p c q", c=4)),
                             reads=[psb[7]], writes=[xb[t // 4]])
                def tail(tb, sbi=sbi):
                    t0 = tb * 512
                    for t in (3, 7):
                        if t // 4 == tb:
                            i_own = sbi * 2 + (1 if t == 7 else 0)
                            P.dma('pool', x1_d[:, :, i_own * 128:(i_own + 1) * 128], xT[:, :, t * 128:(t + 1) * 128], reads=[xb[tb]], writes=[x1db], sem_owner=xstb[tb])
                    rms_fm(xT, xb, G_MIXPRE, hT, hb, 512, sq, sqb, rs, rsb, t_start=t0)
                    for s_ in range(4):
                        pi = s_ % 2
                        for k in range(8):
                            P.op('pe', lambda e, k=k, s_=s_, pi=pi: e.matmul(ps[pi][:], lhsT=wkf[:, k, s_ * 128:(s_ + 1) * 128], rhs=hT[:, k, t0:t0 + 512],
                                                                         start=(k == 0), stop=(k == 7)),
                                 reads=[B_wk, hb[tb]], writes=[psb[pi]])
                        P.op('act', lambda e, s_=s_, pi=pi: e.activation(out=kst[:, s_, t0:t0 + 512], in_=ps[pi][:], func=AF.Copy),
                             reads=[psb[pi]], writes=[kstb[tb][s_]])
                        P.dma('pool', kt_d[s_][:, sbi * SBN + t0:sbi * SBN + t0 + 512], kst[:, s_, t0:t0 + 512], reads=[kstb[tb][s_]], writes=[ktdb[s_]],
                              sem_owner=kstb[tb][s_])
                    for t in range(tb * 4, tb * 4 + 4):
                        pi = 2 + t % 2
                        for k in range(8):
                            P.op('pe', lambda e, k=k, t=t, pi=pi: e.matmul(ps[pi][:, 0:256], lhsT=hT[:, k, t * 128:(t + 1) * 128], rhs=wvt[:, k, :],
                                                                         start=(k == 0), stop=(k == 7)),
                                 reads=[B_wk, hb[tb]], writes=[psb[pi]])
                        P.op('dve', lambda e, t=t, pi=pi: e.tensor_copy(out=vst[:, t, :], in_=ps[pi][:, 0:256]), reads=[psb[pi]], writes=[vstb[tb]])
                    r0 = sbi * SBN + t0
                    P.dma('pool', vt_d[r0:r0 + 512, :].rearrange("(t p) m -> p t m", p=128), vst[:, tb * 4:tb * 4 + 4, :], reads=[vstb[tb]], writes=[vtdb], sem_owner=vstb[tb])

                ffn(ph, 0, xT, xb, hT, hb, actT, actb, yT, yb_, sq, sqb, rs, rsb, sg, sgb, wgs, wus, wds, G_F1PRE, 0, after_block=tail)
            P.barrier()
            P.emit()

        def dbg_exit():
            with ExitStack() as phd:
                tmp = sbuf(phd, "dbg_t", [128, D], F32)
                tb_ = Buf("dbg_t")
                P.op('pool', lambda e: e.memset(tmp[:], 0.0), writes=[tb_])
                for i in range(NOWN):
                    P.dma('sp', y_d[i * 128:(i + 1) * 128, :], tmp[:], reads=[tb_], writes=[Buf("yo%d" % i)], is_output=True)
                P.finish()
                P.emit()
            return nc

        if stage <= 2:
            return dbg_exit()

        att = es.enter_context(ExitStack())
        KcT = sbuf(att, "KcT", [128, 512], BF16)
        Vc = sbuf(att, "Vc", [128, 4, 2, 65], BF16)
        VcN = sbuf(att, "VcN", [16, NOWN, 2, 65], BF16)
        KE = [sbuf(att, "KE%d" % g_, [128, SEQ], BF16) for g_ in range(2)]
        kwnT = sbuf(att, "kwnT", [128, SEQ], BF16)
        vsl = sbuf(att, "vsl", [128, NT, 2, 65], BF16)
        vwn = sbuf(att, "vwn", [128, NT, 2, 65], BF16)
        QT = sbuf(att, "QT", [128, 4, NOWN * 128], BF16)
        sig = sbuf(att, "sig", [128, NOWN, 24], F32)
        B_KcT, B_Vc, B_VcN, B_ksl, B_kwn, B_vsl, B_vwn, B_QT, B_sig = [Buf(n) for n in
                                                                      ("KcT", "Vc", "VcN", "kslT", "kwnT", "vsl", "vwn", "QT", "sig")]
        yn_d = nc.dram_tensor("sc_yn", [128, 8, NOWN * 128], BF16, kind=skind)
        B_ynd = Buf("ynd")
        with ExitStack() as ph:
            rawT = [sbuf(ph, "c_raw%d" % kv, [128, SEQ], BF16) for kv in range(2)]
            w1 = [sbuf(ph, "c_w1%d" % kv, [128, 32, 256], BF16) for kv in range(2)]
            w2x = [sbuf(ph, "c_w2%d" % kv, [128, 2, 128], BF16) for kv in range(2)]
            hid = [sbuf(ph, "c_hid%d" % kv, [128, 2, 2, 512], BF16) for kv in range(2)]
            pe_bf = sbuf(ph, "c_pe", [64, 64], BF16)
            cvec = sbuf(ph, "c_cvec", [128, 2, 2], F32)
            B_raw = [Buf("raw0"), Buf("raw1")]
            B_w1 = [Buf("w10"), Buf("w11")]
            B_hid = [Buf("hid0"), Buf("hid1")]
            B_cv = Buf("cvec")
            B_pe = Buf("pe")
            P.dma('pool', pe_bf[:], peT_d[:, :], writes=[B_pe])
            for kv in range(2):
                P.dma('sp', rawT[kv][:], kt_d[kv][:, :], reads=[ktdb[kv]], writes=[B_raw[kv]])
                for hh in range(2):
                    P.dma('pool', w1[kv][hh * 64:(hh + 1) * 64, :, :], cw1_d[kv][:, :].rearrange("(p d) j -> d p j", d=64), writes=[B_w1[kv]])
                    P.dma('pool', w2x[kv][:, :, hh * 64:(hh + 1) * 64], cw2_d[kv][:, :].rearrange("(jc p) d -> p jc d", p=128), writes=[B_w1[kv]])
                P.op('pool', lambda e, kv=kv: e.memset(hid[kv][:], 0.0), writes=[B_hid[kv]])
            P.op('pool', lambda e: e.memset(Vc[:, :, :, 64:65], 1.0), writes=[B_Vc])
            P.op('pool', lambda e: e.memset(VcN[:, :, :, 64:65], 1.0), writes=[B_VcN])
            for g_ in range(2):
                oth = slice(64, 128) if g_ == 0 else slice(0, 64)
                P.op('pool', lambda e, g_=g_, oth=oth: e.memset(KE[g_][oth, :], 1.0), writes=[B_ksl])
                for cmp_base, pat, cm in ((0, [[0, 2], [128, 32], [1, 128]], -64), (63, [[0, 2], [-128, 32], [-1, 128]], 64)):
                    P.op('pool', lambda e, g_=g_, oth=oth, cmp_base=cmp_base, pat=pat, cm=cm: e.affine_select(
                        out=KE[g_][oth, :].rearrange("p (a b k) -> p a b k", a=2, b=32), in_=KE[g_][oth, :].rearrange("p (a b k) -> p a b k", a=2, b=32),
                        pattern=pat, compare_op=ALU.is_ge, fill=0.0, base=cmp_base, channel_multiplier=cm), reads=[B_ksl], writes=[B_ksl])
                P.dma('sp', KE[g_][64 * g_:64 * g_ + 64, :], kt_d[2][64 * g_:64 * g_ + 64, :], reads=[ktdb[2], B_ksl], writes=[B_ksl])
            P.dma('sp', kwnT[:], kt_d[3][:, :], reads=[ktdb[3]], writes=[B_kwn])
            P.op('pool', lambda e: e.memset(vsl[:, :, :, 64:65], 1.0), writes=[B_vsl])
            P.op('pool', lambda e: e.memset(vwn[:, :, :, 64:65], 1.0), writes=[B_vwn])
            for g in range(2):
                P.dma('sp', vsl[:, :, g, 0:64], vt_d[:, g * 64:(g + 1) * 64].rearrange("(t p) d -> p t d", p=128), reads=[vtdb, B_vsl], writes=[B_vsl])
                P.dma('sp', vwn[:, :, g, 0:64], vt_d[:, 128 + g * 64:128 + (g + 1) * 64].rearrange("(t p) d -> p t d", p=128), reads=[vtdb, B_vwn], writes=[B_vwn])

            for kv in range(2):
                for jc in range(2):
                    for p_ in range(32):
                        P.op('pe', lambda e, kv=kv, jc=jc, p_=p_: e.matmul(ps[0][:, jc:jc + 1], lhsT=w1[kv][0:64, p_, jc * 128:(jc + 1) * 128],
                                                                         rhs=pe_bf[0:64, kv * 32 + p_:kv * 32 + p_ + 1], start=(p_ == 0), stop=(p_ == 31)),
                             reads=[B_w1[kv], B_pe], writes=[psb[0]])
                gofs = G_CKB1 if kv == 0 else G_CVB1
                P.op('dve', lambda e, kv=kv, gofs=gofs: e.tensor_tensor(out=cvec[:, kv, :], in0=ps[0][:, 0:2], in1=gcol[:, gofs:gofs + 2], op=ALU.add),
                     reads=[psb[0], B_gcol], writes=[B_cv])
                r3 = rawT[kv][:, :].rearrange("q (c s) -> q c s", s=16)
                for g in range(2):
                    for jc in range(2):
                        hi = 1 + (g * 2 + jc) % 2
                        for p_ in range(32):
                            rhs = r3[64 * g:64 * g + 64, 0:511, p_] if p_ < 16 else r3[64 * g:64 * g + 64, 1:512, p_ - 16]
                            P.op('pe', lambda e, kv=kv, g=g, jc=jc, p_=p_, rhs=rhs, hi=hi: e.matmul(
                                ps[hi][:, 0:511], lhsT=w1[kv][64 * g:64 * g + 64, p_, jc * 128:(jc + 1) * 128], rhs=rhs, start=(p_ == 0), stop=(p_ == 31)),
                                reads=[B_w1[kv], B_raw[kv]], writes=[psb[hi]])
                        P.op('act', lambda e, kv=kv, g=g, jc=jc, hi=hi: e.activation(out=hid[kv][:, jc, g, 0:511], in_=ps[hi][:, 0:511], func=AF.Gelu,
                                                                                 bias=cvec[:, kv, jc:jc + 1]),
                             reads=[psb[hi], B_cv], writes=[B_hid[kv]])
            for g in range(2):
                for jc in range(2):
                    P.op('pe', lambda e, g=g, jc=jc: e.matmul(ps[3][:, 0:511], lhsT=w2x[0][:, jc, :], rhs=hid[0][:, jc, g, 0:511], start=(jc == 0), stop=(jc == 1)),
                         reads=[B_w1[0], B_hid[0]], writes=[psb[3]])
                P.op('act', lambda e, g=g: e.activation(out=KcT[64 * g:64 * g + 64, 0:511], in_=ps[3][64 * g:64 * g + 64, 0:511], func=AF.Copy),
                     reads=[psb[3]], writes=[B_KcT])
                for ct in range(4):
                    for jc in range(2):
                        P.op('pe', lambda e, g=g, jc=jc, ct=ct: e.matmul(ps[4][:, 0:64], lhsT=hid[1][:, jc, g, ct * 128:(ct + 1) * 128], rhs=w2x[1][:, jc, 0:64],
                                                                       start=(jc == 0), stop=(jc == 1)),
                             reads=[B_w1[1], B_hid[1]], writes=[psb[4]])
                    P.op('dve', lambda e, g=g, ct=ct: e.tensor_copy(out=Vc[:, ct, g, 0:64], in_=ps[4][:, 0:64]), reads=[psb[4]], writes=[B_Vc])
                for i in range(NOWN):
                    c0 = 32 * i + 15
                    for jc in range(2):
                        P.op('pe', lambda e, g=g, jc=jc, c0=c0: e.matmul(ps[5][0:16, 0:64], lhsT=hid[1][:, jc, g, c0:c0 + 16], rhs=w2x[1][:, jc, 0:64],
                                                                       start=(jc == 0), stop=(jc == 1)),
                             reads=[B_w1[1], B_hid[1]], writes=[psb[5]])
                    P.op('dve', lambda e, g=g, i=i: e.tensor_copy(out=VcN[0:16, i, g, 0:64], in_=ps[5][0:16, 0:64]), reads=[psb[5]], writes=[B_VcN])
            P.barrier()
            P.emit()

        with ExitStack() as ph:
            wu_sb = sbuf(ph, "a_wu", [128, 8, 512], BF16)
            wv_sb = sbuf(ph, "a_wv", [128, 8, 512], BF16)
            wq_sb = sbuf(ph, "a_wq", [128, 8, 512], BF16)
            wgt_sb = sbuf(ph, "a_wgt", [128, 8, 24], BF16)
            wsr = sbuf(ph, "a_wsr", [128, 4, 128], F32)
            wtf = sbuf(ph, "a_wtf", [128, 4, 128], F32)
            WT = sbuf(ph, "a_WT", [128, 4, 128], BF16)
            LNG = sbuf(ph, "a_lng", [128, 512], F32)
            LNB = sbuf(ph, "a_lnb", [128, 512], F32)
            BSb = sbuf(ph, "a_bs", [128, 512], F32)
            x1t = [sbuf(ph, "a_x1t%d" % i, [128, 8, 128], F32) for i in range(2)]
            h2t_2 = [sbuf(ph, "a_h2t%d" % k_, [128, 8, 128], BF16) for k_ in range(2)]
            sq = sbuf(ph, "a_sq", [128, 8, 512], BF16)
            rs = sbuf(ph, "a_rs", [128, 512], F32)
            uT_2 = [sbuf(ph, "a_uT%d" % k_, [128, 512], F32) for k_ in range(2)]
            vg_2 = [sbuf(ph, "a_vg%d" % k_, [128, 512], F32) for k_ in range(2)]
            vcen_2 = [sbuf(ph, "a_vcen%d" % k_, [128, 512], F32) for k_ in range(2)]
            vsq_2 = [sbuf(ph, "a_vsq%d" % k_, [128, 512], F32) for k_ in range(2)]
            vln_2 = [sbuf(ph, "a_vln%d" % k_, [128, 512], BF16) for k_ in range(2)]
            ya_2 = [sbuf(ph, "a_ya%d" % k_, [128, 512], F32) for k_ in range(2)]
            yan_2 = [sbuf(ph, "a_yan%d" % k_, [128, 4, 128], BF16) for k_ in range(2)]
            st4_2 = [sbuf(ph, "a_st4%d" % k_, [128, 16], F32) for k_ in range(2)]
            B_w = Buf("a_w")
            B_wsr, B_WT = Buf("wsr"), Buf("WT")
            B_x1t = [Buf("x1t0"), Buf("x1t1")]
            B_rs = Buf("rs")
            B2 = {n: [Buf(n + "0"), Buf(n + "1")] for n in ("h2t", "uT", "vg", "vcen", "vsq", "vln", "ya", "yan", "st4")}
            sqb = [Buf("asq%d" % i) for i in range(8)]
            P.dma('pool', wu_sb[:], win_d[:, C_U:C_U + 512].rearrange("(kc p) m -> p kc m", p=128), writes=[B_w])
            P.dma('pool', wv_sb[:], win_d[:, C_V:C_V + 512].rearrange("(kc p) m -> p kc m", p=128), writes=[B_w])
            P.dma('pool', wq_sb[:], win_d[:, C_Q:C_Q + 512].rearrange("(kc p) m -> p kc m", p=128), writes=[B_w])
            P.dma('pool', wgt_sb[:], win_d[:, C_GT:C_GT + 24].rearrange("(kc p) m -> p kc m", p=128), writes=[B_w])
            P.dma('sp', wsr[:], ws_d[:, :, :].rearrange("g p q -> p g q"), writes=[B_wsr])
            B_w2 = Buf("a_w2")
            P.dma('sp', LNG[:], rowv_d[0:1, R_LNG:R_LNG + 512].partition_broadcast(128), writes=[B_w2])
            P.dma('sp', LNB[:], rowv_d[0:1, R_LNB:R_LNB + 512].partition_broadcast(128), writes=[B_w2])
            P.dma('sp', BSb[:], rowv_d[0:1, R_BS:R_BS + 512].partition_broadcast(128), writes=[B_w2])
            for g in range(4):
                P.op('pe', lambda e, g=g: e.transpose(ps[7][:, g * 128:(g + 1) * 128], wsr[:, g, :], ident[:]), reads=[B_wsr, B_const], writes=[psb[7]])
            P.op('dve', lambda e: e.tensor_copy(out=wtf[:].rearrange("p g q -> p (g q)"), in_=ps[7][:]), reads=[psb[7]], writes=[B_WT])
            P.op('pool', lambda e: e.affine_select(out=WT[:], in_=wtf[:], pattern=[[0, 4], [1, 128]], compare_op=ALU.is_ge, fill=0.0,
                                                   base=0, channel_multiplier=-1), reads=[B_WT], writes=[B_WT])

            def tile_front(i):
                xi = i % 2
                tsl = slice(i * 128, (i + 1) * 128)
                h2t, uT, vg, vcen, vsq, vln, ya, yan, st4 = [d_[xi] for d_ in (h2t_2, uT_2, vg_2, vcen_2, vsq_2, vln_2, ya_2, yan_2, st4_2)]
                B_h2t, B_uT, B_vg, B_vcen, B_vsq, B_vln, B_ya, B_yan, B_st4 = [B2[n][xi] for n in ("h2t", "uT", "vg", "vcen", "vsq", "vln", "ya", "yan", "st4")]
                P.dma('sp', x1t[xi][:], x1_d[:, :, tsl], reads=[x1db], writes=[B_x1t[xi]], parallel=False)
                rms_fm(x1t[xi], B_x1t[xi], G_MIXPRE, h2t, B_h2t, 128, sq, sqb, rs, B_rs)
                for r in range(4):
                    pi = r % 2
                    for k in range(8):
                        P.op('pe', lambda e, k=k, r=r, pi=pi: e.matmul(ps[pi][:, 0:128], lhsT=wq_sb[:, k, r * 128:(r + 1) * 128], rhs=h2t[:, k, :],
                                                                     start=(k == 0), stop=(k == 7)), reads=[B_w, B_h2t], writes=[psb[pi]])
                    P.op('act', lambda e, r=r, pi=pi, tsl=tsl: e.activation(out=QT[:, r, tsl], in_=ps[pi][:, 0:128], func=AF.Copy), reads=[psb[pi]], writes=[B_QT])
                for k in range(8):
                    P.op('pe', lambda e, k=k: e.matmul(ps[2][:, 0:24], lhsT=h2t[:, k, :], rhs=wgt_sb[:, k, :], start=(k == 0), stop=(k == 7)),
                         reads=[B_w, B_h2t], writes=[psb[2]])
                P.op('act', lambda e, i=i: e.activation(out=sig[:, i, :], in_=ps[2][:, 0:24], func=AF.Sigmoid), reads=[psb[2]], writes=[B_sig])
                for g in range(4):
                    for k in range(8):
                        P.op('pe', lambda e, k=k, g=g: e.matmul(ps[3][:, g * 128:(g + 1) * 128], lhsT=wu_sb[:, k, g * 128:(g + 1) * 128], rhs=h2t[:, k, :],
                                                              start=(k == 0), stop=(k == 7)), reads=[B_w, B_h2t], writes=[psb[3]])
                P.op('act', lambda e: e.activation(out=uT[:], in_=ps[3][:], func=AF.Gelu), reads=[psb[3]], writes=[B_uT])
                for k in range(8):
                    P.op('pe', lambda e, k=k: e.matmul(ps[4][:], lhsT=h2t[:, k, :], rhs=wv_sb[:, k, :], start=(k == 0), stop=(k == 7)),
                         reads=[B_w, B_h2t], writes=[psb[4]])
                P.op('act', lambda e: e.activation(out=vg[:], in_=ps[4][:], func=AF.Gelu), reads=[psb[4]], writes=[B_vg])

            def tile_back(i):
                xi = i % 2
                tsl = slice(i * 128, (i + 1) * 128)
                h2t, uT, vg, vcen, vsq, vln, ya, yan, st4 = [d_[xi] for d_ in (h2t_2, uT_2, vg_2, vcen_2, vsq_2, vln_2, ya_2, yan_2, st4_2)]
                B_h2t, B_uT, B_vg, B_vcen, B_vsq, B_vln, B_ya, B_yan, B_st4 = [B2[n][xi] for n in ("h2t", "uT", "vg", "vcen", "vsq", "vln", "ya", "yan", "st4")]
                vg3 = vg[:].rearrange("p (g c) -> p g c", g=4)
                vc3 = vcen[:].rearrange("p (g c) -> p g c", g=4)
                vs3 = vsq[:].rearrange("p (g c) -> p g c", g=4)
                P.op('dve', lambda e: e.tensor_reduce(out=st4[:, 0:4], in_=vg3, axis=AX.X, op=ALU.add), reads=[B_vg], writes=[B_st4])
                P.op('dve', lambda e: e.tensor_scalar(out=st4[:, 4:8], in0=st4[:, 0:4], scalar1=-1.0 / 128, scalar2=None, op0=ALU.mult), reads=[B_st4], writes=[B_st4])
                P.op('dve', lambda e: e.tensor_tensor(out=vc3, in0=vg3, in1=st4[:, 4:8].unsqueeze(2).to_broadcast([128, 4, 128]), op=ALU.add),
                     reads=[B_vg, B_st4], writes=[B_vcen])
                P.op('act', lambda e: e.activation(out=vsq[:], in_=vcen[:], func=AF.Square), reads=[B_vcen], writes=[B_vsq])
                P.op('dve', lambda e: e.tensor_reduce(out=st4[:, 8:12], in_=vs3, axis=AX.X, op=ALU.add), reads=[B_vsq, B_st4], writes=[B_st4])
                P.op('act', lambda e: e.activation(out=st4[:, 12:16], in_=st4[:, 8:12], func=AF.Sqrt, scale=1.0 / 128, bias=EPS), reads=[B_st4], writes=[B_st4])
                P.op('dve', lambda e: e.reciprocal(out=st4[:, 12:16], in_=st4[:, 12:16]), reads=[B_st4], writes=[B_st4])
                P.op('dve', lambda e: e.tensor_tensor(out=vs3, in0=vc3, in1=st4[:, 12:16].unsqueeze(2).to_broadcast([128, 4, 128]), op=ALU.mult),
                     reads=[B_vcen, B_st4, B_vsq], writes=[B_vsq])
                P.op('dve', lambda e: e.tensor_tensor(out=vsq[:], in0=vsq[:], in1=LNG[:], op=ALU.mult), reads=[B_vsq, B_w2], writes=[B_vsq])
                P.op('dve', lambda e: e.tensor_tensor(out=vln[:], in0=vsq[:], in1=LNB[:], op=ALU.add), reads=[B_vsq, B_w2], writes=[B_vln])
                for g in range(4):
                    P.op('pe', lambda e, g=g: e.matmul(ps[5][:, g * 128:(g + 1) * 128], lhsT=vln[:, g * 128:(g + 1) * 128], rhs=WT[:, g, :], start=True, stop=True),
                         reads=[B_vln, B_WT], writes=[psb[5]])
                P.op('dve', lambda e: e.tensor_tensor(out=ya[:], in0=ps[5][:], in1=BSb[:], op=ALU.add), reads=[psb[5], B_w2], writes=[B_ya])
                P.op('dve', lambda e: e.tensor_tensor(out=ya[:], in0=ya[:], in1=uT[:], op=ALU.mult), reads=[B_ya, B_uT], writes=[B_ya])
                for g in range(4):
                    P.op('act', lambda e, g=g: e.activation(out=sq[:, g, 0:128], in_=ya[:, g * 128:(g + 1) * 128], func=AF.Square), reads=[B_ya], writes=[sqb[g]])
                    P.op('pe', lambda e, g=g: e.matmul(ps[6][:, 0:128], lhsT=ones_bf[:], rhs=sq[:, g, 0:128], start=(g == 0), stop=(g == 3)),
                         reads=[sqb[g], B_const], writes=[psb[6]])
                rstd_from_stat(None, ps[6], psb[6], rs, B_rs, 512.0, 128)
                for g in range(4):
                    P.op('dve', lambda e, g=g: e.scalar_tensor_tensor(out=yan[:, g, :], in0=ya[:, g * 128:(g + 1) * 128], scalar=gcol[:, G_OGA + g:G_OGA + g + 1],
                                                                    in1=rs[:, 0:128], op0=ALU.mult, op1=ALU.mult),
                         reads=[B_ya, B_rs, B_gcol], writes=[B_yan])
                P.dma('sp', yn_d[:, 0:4, tsl], yan[:], reads=[B_yan], writes=[B_ynd], sem_owner=B_yan)
            tile_front(0)
            for i in range(NOWN):
                if i + 1 < NOWN:
                    tile_front(i + 1)
                tile_back(i)
            P.barrier()
            P.emit()

        if stage <= 3:
            if debug:
                dq = nc.dram_tensor("dbg_qt", [128, 4 * NOWN * 128], BF16, kind="ExternalOutput")
                dsg = nc.dram_tensor("dbg_sig", [128, NOWN * 24], F32, kind="ExternalOutput")
                dk = nc.dram_tensor("dbg_kct", [128, 512], BF16, kind="ExternalOutput")
                dv = nc.dram_tensor("dbg_vc", [128, 4 * 2 * 65], BF16, kind="ExternalOutput")
                dvn = nc.dram_tensor("dbg_vcn", [16, NOWN * 2 * 65], BF16, kind="ExternalOutput")
                P.dma('sp', dq[:, :], QT[:].rearrange("p r q -> p (r q)"), reads=[B_QT], writes=[Buf("dq")], is_output=True)
                P.dma('sp', dsg[:, :], sig[:].rearrange("p i c -> p (i c)"), reads=[B_sig], writes=[Buf("dsg")], is_output=True)
                P.dma('sp', dk[:, :], KcT[:], reads=[B_KcT], writes=[Buf("dk")], is_output=True)
                P.dma('sp', dv[:, :], Vc[:].rearrange("p a b c -> p (a b c)"), reads=[B_Vc], writes=[Buf("dv")], is_output=True)
                P.dma('sp', dvn[:, :], VcN[:].rearrange("p a b c -> p (a b c)"), reads=[B_VcN], writes=[Buf("dvn")], is_output=True)
                P.barrier()
                P.emit()
            att.close()
            return dbg_exit()

        with ExitStack() as ph:
            tab_sb = sbuf(ph, "b_tab", [32, 8], F32)
            negrow = sbuf(ph, "b_neg", [1, 8], F32)
            ohd_sb = sbuf(ph, "b_ohd", [32, 768], F32)
            ohm_sb = sbuf(ph, "b_ohm", [1, 768], F32)
            fv_sb = sbuf(ph, "b_fv", [8, 768], F32)
            Hs = sbuf(ph, "b_Hs", [128, 8, 3, 128], F32)
            H2s = sbuf(ph, "b_H2s", [16, 8, 128], F32)
            BIAS = sbuf(ph, "b_BIAS", [128, 2, 3, 512], F32)
            BN = sbuf(ph, "b_BN", [16, 2, 512], F32)
            RQ = [[sbuf(ph, "b_RQ%d%d" % (a_, b_), [128, 4, 128], BF16) for b_ in range(2)] for a_ in range(2)]
            B_RQ = [[Buf("RQ%d%d" % (a_, b_)) for b_ in range(2)] for a_ in range(2)]
            selsw = sbuf(ph, "b_selsw", [128, 128], F32)
            B_selsw = Buf("selsw")
            SMASK = sbuf(ph, "b_smask", [128, NOWN, 2, 128], F32)
            KB = sbuf(ph, "b_kb", [128, NT], F32)
            KBC = sbuf(ph, "b_kbc", [128, 4], F32)
            KBN = sbuf(ph, "b_kbn", [16, NOWN], F32)
            OVF = sbuf(ph, "b_ovf", [128, 4, 128], BF16)
            OVN = sbuf(ph, "b_ovn", [16, NOWN, 128], BF16)
            OGB = sbuf(ph, "b_ogb", [128, 512], F32)
            Pt = [sbuf(ph, "b_Pt%d" % i, [128, 512], BF16) for i in range(4)]
            tmpS = [sbuf(ph, "b_tmpS%d" % i, [128, 512], F32) for i in range(2)]
            rsbt = sbuf(ph, "b_rsb", [128, 512], F32)
            imp4 = sbuf(ph, "b_imp4", [128, 512], F32)
            impT = sbuf(ph, "b_impT", [128, 128], F32)
            score = sbuf(ph, "b_score", [128, 128], F32)
            sc2 = sbuf(ph, "b_sc2", [128, 128], F32)
            selt = sbuf(ph, "b_sel", [128, 128], F32)
            m8 = sbuf(ph, "b_m8", [128, 24], F32)
            negsel = sbuf(ph, "b_negsel", [128, 4, 128], BF16)
            cf = sbuf(ph, "b_cf", [128, 16], F32)
            ybt = sbuf(ph, "b_yb", [128, 8, 64], F32)
            ybn = sbuf(ph, "b_ybn", [128, 512], F32)
            tmpo = sbuf(ph, "b_tmpo", [128, 4, 64], F32)
            ybT = sbuf(ph, "b_ybT", [128, 4, 128], BF16)
            B_c2 = Buf("b_const")
            B_fv, B_fvd, B_Hs, B_BIAS = Buf("fv"), Buf("fvd"), Buf("Hs"), Buf("BIAS")
            B_Pt = [Buf("Pt%d" % i) for i in range(4)]
            B_tmpS = [Buf("tmpS0"), Buf("tmpS1")]
            B_rsb, B_imp4, B_impT, B_score, B_sc2, B_sel, B_m8, B_negsel, B_cf, B_yb, B_ybn, B_tmpo, B_ybT = [Buf(n) for n in (
                "rsb", "imp4", "impT", "score", "sc2", "sel", "m8", "negsel", "cf", "yb", "ybn", "tmpo", "ybT")]
            P.dma('sp', tab_sb[:], tab_d[:, :], writes=[B_c2])
            P.dma('sp', ohd_sb[:], ohd_d[0:32, :], writes=[B_c2])
            P.dma('sp', ohm_sb[:], ohd_d[32:33, :], writes=[B_c2])
            P.dma('sp', SMASK[:], smask_d[:, :, :, :], writes=[B_c2])
            P.dma('sp', KB[:], kb_d[:, :], writes=[B_c2])
            P.dma('sp', KBC[:], kbc_d[:, :], writes=[B_c2])
            P.dma('sp', KBN[:], kbn_d[:, :], writes=[B_c2])
            B_c2p = Buf("b_constp")
            P.dma('pool', OVF[:], ovf_d[:, :, :], writes=[B_c2p])
            P.dma('pool', OVN[:], ovn_d[:, :, :], writes=[B_c2p])
            P.dma('sp', OGB[:], rowv_d[0:1, R_OGB:R_OGB + 512].partition_broadcast(128), writes=[B_c2])
            P.op('pool', lambda e: e.memset(negrow[:], NEG), writes=[B_c2])
            for half in range(2):
                cs = slice(half * 384, (half + 1) * 384)
                P.op('pe', lambda e, cs=cs: e.matmul(ps[0][0:8, 0:384], lhsT=tab_sb[:, :], rhs=ohd_sb[:, cs], start=True, stop=False), reads=[B_c2], writes=[psb[0]])
                P.op('pe', lambda e, cs=cs: e.matmul(ps[0][0:8, 0:384], lhsT=negrow[:, :], rhs=ohm_sb[:, cs], start=False, stop=True), reads=[B_c2], writes=[psb[0]])
                P.op('dve', lambda e, cs=cs: e.tensor_copy(out=fv_sb[:, cs], in_=ps[0][0:8, 0:384]), reads=[psb[0]], writes=[B_fv])
            P.dma('sp', fv_d[:, :], fv_sb[:], reads=[B_fv], writes=[B_fvd])
            for h in range(8):
                for di, dl in enumerate((0, 1, 4)):
                    src = bass.AP(tensor=fv_d, offset=h * 768 + dl * 128, ap=[[1, 128], [1, 128]])
                    P.dma('sp', Hs[:, h, di, :], src, reads=[B_fvd], writes=[B_Hs])
                src = bass.AP(tensor=fv_d, offset=h * 768, ap=[[16, 16], [1, 128]])
                P.dma('sp', H2s[:, h, :], src, reads=[B_fvd], writes=[B_Hs])
            for g in range(2):
                for di in range(3):
                    pi = (g * 3 + di) % 2
                    P.op('pe', lambda e, g=g, di=di, pi=pi: e.matmul(ps[pi][:], lhsT=antid[:, :], rhs=Hs[:, 4 * g:4 * g + 4, di, :], start=True, stop=True),
                         reads=[B_Hs, B_const], writes=[psb[pi]])
                    P.op('act', lambda e, g=g, di=di, pi=pi: e.activation(out=BIAS[:, g, di, :], in_=ps[pi][:], func=AF.Copy), reads=[psb[pi]], writes=[B_BIAS])
                P.op('pe', lambda e, g=g: e.matmul(ps[2][0:16, :], lhsT=antid[0:16, 112:128], rhs=H2s[0:16, 4 * g:4 * g + 4, :], start=True, stop=True),
                     reads=[B_Hs, B_const], writes=[psb[2]])
                P.op('act', lambda e, g=g: e.activation(out=BN[0:16, g, :], in_=ps[2][0:16, :], func=AF.Copy), reads=[psb[2]], writes=[B_BIAS])

            state = {'pt': 0, 'tmp': 0, 's': 0}
            NPT = 4

            def unit(i, g, kT_ap, w, v_ap, kvbufs, o_i, first, last, bias_ap=None, kbias_ap=None, mask_kt=None, ov_ap=None, scale=SCALE, sbanks=(0, 1)):
                si = sbanks[state['s'] % len(sbanks)]
                state['s'] += 1
                pti = state['pt'] % NPT
                state['pt'] += 1
                if mask_kt is None:
                    qrhs = QT[64 * g:64 * g + 64, :, i * 128:(i + 1) * 128]
                    qb_ = [B_QT]
                else:
                    rq_a, rq_b = mask_kt
                    qrhs = RQ[rq_a][rq_b][:]
                    qb_ = [B_RQ[rq_a][rq_b]]
                P.op('pe', lambda e: e.matmul(ps[si][:w, :], lhsT=kT_ap, rhs=qrhs, start=True, stop=True), reads=kvbufs + qb_, writes=[psb[si]])
                kb_ = kbias_ap if kbias_ap is not None else 0.0
                if bias_ap is not None:
                    ti = state['tmp'] % 2
                    state['tmp'] += 1
                    P.op('dve', lambda e: e.scalar_tensor_tensor(out=tmpS[ti][:w, :], in0=ps[si][:w, :], scalar=scale, in1=bias_ap, op0=ALU.mult, op1=ALU.add),
                         reads=[psb[si], B_BIAS], writes=[B_tmpS[ti]])
                    P.op('act', lambda e: e.activation(out=Pt[pti][:w, :], in_=tmpS[ti][:w, :], func=AF.Exp, bias=kb_), reads=[B_tmpS[ti], B_c2, B_const], writes=[B_Pt[pti]])
                else:
                    P.op('act', lambda e: e.activation(out=Pt[pti][:w, :], in_=ps[si][:w, :], func=AF.Exp, scale=scale, bias=kb_),
                         reads=[psb[si], B_c2, B_const], writes=[B_Pt[pti]])
                def pv():
                    for r in range(4):
                        P.op('pe', lambda e, r=r: e.matmul(ps[o_i][:, r * 65:(r + 1) * 65], lhsT=Pt[pti][:w, r * 128:(r + 1) * 128], rhs=v_ap, start=(first and r == 0), stop=last, skip_group_check=True),
                             reads=[B_Pt[pti]] + kvbufs, writes=[psb[o_i]])
                    if ov_ap is not None:
                        P.op('pe', lambda e: e.matmul(ps[5][:], lhsT=ov_ap, rhs=Pt[pti][:w, :], start=first, stop=last), reads=[B_Pt[pti], B_c2p], writes=[psb[5]])
                        P.op('pe', lambda e: e.matmul(ps[6][:], lhsT=ones_bf[:w, :], rhs=Pt[pti][:w, :], start=first, stop=last), reads=[B_Pt[pti], B_const], writes=[psb[6]])
                return pv

            def run_units(specs, skew=1, hooks=None):
                pend = []
                for n_, (args, kw) in enumerate(specs):
                    pend.append(unit(*args, **kw))
                    if len(pend) > skew:
                        pend.pop(0)()
                    if hooks and n_ in hooks:
                        hooks[n_]()
                while pend:
                    pend.pop(0)()

            def combine(i, g, o_i, br, first_branch):
                ybt, B_yb = ybt2[i % 2], B_yb2[i % 2]
                o3 = ps[o_i][:, 0:260].rearrange("p (r e) -> p r e", e=65)
                gate = sig[:, i, :].rearrange("p (h b) -> p h b", b=3)[:, 4 * g:4 * g + 4, br]
                cs = slice(br * 4, br * 4 + 4)
                P.op('dve', lambda e: e.tensor_scalar(out=cf[:, cs], in0=o3[:, :, 64], scalar1=1e-30, scalar2=None, op0=ALU.max), reads=[psb[o_i]], writes=[B_cf])
                P.op('dve', lambda e: e.reciprocal(out=cf[:, cs], in_=cf[:, cs]), reads=[B_cf], writes=[B_cf])
                P.op('dve', lambda e: e.tensor_tensor(out=cf[:, cs], in0=cf[:, cs], in1=gate, op=ALU.mult), reads=[B_cf, B_sig], writes=[B_cf])
                cb = cf[:, cs].unsqueeze(2).to_broadcast([128, 4, 64])
                if first_branch:
                    P.op('dve', lambda e: e.tensor_tensor(out=ybt[:, 4 * g:4 * g + 4, :], in0=o3[:, :, 0:64], in1=cb, op=ALU.mult), reads=[psb[o_i], B_cf], writes=[B_yb])
                else:
                    P.op('dve', lambda e: e.tensor_tensor(out=tmpo[:], in0=o3[:, :, 0:64], in1=cb, op=ALU.mult), reads=[psb[o_i], B_cf], writes=[B_tmpo])
                    P.op('dve', lambda e: e.tensor_tensor(out=ybt[:, 4 * g:4 * g + 4, :], in0=ybt[:, 4 * g:4 * g + 4, :], in1=tmpo[:], op=ALU.add),
                         reads=[B_tmpo, B_yb], writes=[B_yb])

            n_own = NOWN if stage >= 5 else 2
            ybt2 = [ybt, sbuf(ph, "b_yb1", [128, 8, 64], F32)]
            B_yb2 = [B_yb, Buf("yb1")]

            def cw_units(i, g):
                qt = 4 * i + 3
                gs = slice(64 * g, 64 * g + 64)
                for b_ in range(2):
                    P.op('pool', lambda e, b_=b_: e.tensor_copy(out=RQ[(2 * i + g) % 2][b_][gs, :, :], in_=QT[gs, :, i * 128:(i + 1) * 128]),
                         reads=[B_QT, B_RQ[(2 * i + g) % 2][b_]], writes=[B_RQ[(2 * i + g) % 2][b_]])
                nfar = 8 * qt - 9
                c0 = nfar
                tiles = [(ct, 128) for ct in range(nfar // 128)]
                if nfar % 128:
                    tiles.append((nfar // 128, nfar % 128))
                specs = []
                for n_, (ct, w) in enumerate(tiles):
                    specs.append(((i, g, KcT[gs, ct * 128:ct * 128 + w], w, Vc[:w, ct, g, :], [B_KcT, B_Vc], 2, n_ == 0, False),
                                  dict(kbias_ap=KBC[:w, ct:ct + 1], ov_ap=OVF[:w, ct, :])))
                specs.append(((i, g, KcT[gs, c0:c0 + 16], 16, VcN[0:16, i, g, :], [B_KcT, B_VcN], 2, False, True),
                              dict(bias_ap=BN[0:16, g, :], kbias_ap=KBN[0:16, i:i + 1], ov_ap=OVN[0:16, i, :])))
                wl = [dl for dl in (4, 3, 2, 1, 0) if qt - dl >= 0]
                for n_, dl in enumerate(wl):
                    kt = qt - dl
                    bias_ap = BIAS[:, g, {0: 0, 1: 1, 4: 2}[dl], :] if dl in (0, 1, 4) else None
                    specs.append(((i, g, kwnT[gs, kt * 128:(kt + 1) * 128], 128, vwn[:, kt, g, :], [B_kwn, B_vwn], 4, n_ == 0, n_ == len(wl) - 1),
                                  dict(bias_ap=bias_ap, kbias_ap=KB[:, kt:kt + 1])))
                run_units(specs)

            def chain_a(i, g):
                P.op('dve', lambda e: e.tensor_scalar(out=rsbt[:], in0=ps[6][:], scalar1=1e-18, scalar2=None, op0=ALU.max), reads=[psb[6]], writes=[B_rsb])
                P.op('act', lambda e: e.activation(out=rsbt[:], in_=rsbt[:], func=AF.Ln), reads=[B_rsb], writes=[B_rsb])
                P.op('act', lambda e: e.activation(out=rsbt[:], in_=rsbt[:], func=AF.Exp, scale=-1.0), reads=[B_rsb], writes=[B_rsb])
                P.op('dve', lambda e: e.tensor_tensor(out=imp4[:], in0=ps[5][:], in1=rsbt[:], op=ALU.mult), reads=[psb[5], B_rsb], writes=[B_imp4])
                P.op('dve', lambda e: e.tensor_reduce(out=impT[:], in_=imp4[:].rearrange("p (r q) -> p q r", r=4), axis=AX.X, op=ALU.add),
                     reads=[B_imp4], writes=[B_impT])

            def chain_t1(i, g):
                P.op('pe', lambda e: e.transpose(ps[7][:, 0:128], impT[:], ident[:]), reads=[B_impT, B_const], writes=[psb[7]])
                P.op('dve', lambda e: e.tensor_tensor(out=score[:], in0=ps[7][:, 0:128], in1=SMASK[:, i, 0, :], op=ALU.mult), reads=[psb[7], B_c2], writes=[B_score])
                P.op('dve', lambda e: e.tensor_tensor(out=score[:], in0=score[:], in1=SMASK[:, i, 1, :], op=ALU.add), reads=[B_score, B_c2], writes=[B_score])
                P.op('dve', lambda e: e.max(out=m8[:, 0:8], in_=score[:]), reads=[B_score], writes=[B_m8])
                P.op('dve', lambda e: e.match_replace(out=sc2[:], in_to_replace=m8[:, 0:8], in_values=score[:], imm_value=-1e30), reads=[B_score, B_m8], writes=[B_sc2])
                P.op('dve', lambda e: e.max(out=m8[:, 8:16], in_=sc2[:]), reads=[B_sc2, B_m8], writes=[B_m8])
                P.op('dve', lambda e: e.tensor_scalar(out=m8[:, 16:17], in0=m8[:, 15:16], scalar1=0.0, scalar2=None, op0=ALU.max), reads=[B_m8], writes=[B_m8])
                P.op('dve', lambda e: e.tensor_scalar(out=selt[:], in0=score[:], scalar1=m8[:, 16:17], scalar2=None, op0=ALU.is_ge), reads=[B_score, B_m8], writes=[B_sel])
                P.op('dve', lambda e: e.tensor_copy(out=selsw[:, 0:64], in_=selt[:, 64:128]), reads=[B_sel], writes=[B_selsw])
                P.op('dve', lambda e: e.tensor_copy(out=selsw[:, 64:128], in_=selt[:, 0:64]), reads=[B_sel, B_selsw], writes=[B_selsw])

            def chain_t23(i, g):
                rqa = (2 * i + g) % 2
                oth = slice(64, 128) if g == 0 else slice(0, 64)
                P.op('pe', lambda e: e.transpose(ps[7][:, 128:256], selt[:], ident[:]), reads=[B_sel, B_const], writes=[psb[7]])
                P.op('pe', lambda e: e.transpose(ps[7][:, 256:384], selsw[:], ident[:]), reads=[B_selsw, B_const], writes=[psb[7]])
                src_lo = ps[7][oth, 256:384] if g == 0 else ps[7][oth, 128:256]
                src_hi = ps[7][oth, 128:256] if g == 0 else ps[7][oth, 256:384]
                for b_, src_ in ((0, src_lo), (1, src_hi)):
                    P.op('dve', lambda e, b_=b_, src_=src_: e.tensor_scalar(
                        out=RQ[rqa][b_][oth, :, :], in0=src_.unsqueeze(1).to_broadcast([64, 4, 128]), scalar1=-1.0, scalar2=BIGNEG, op0=ALU.add, op1=ALU.mult),
                        reads=[psb[7], B_RQ[rqa][b_]], writes=[B_RQ[rqa][b_]])

            def sel_units(i, g, hooks):
                qt = 4 * i + 3
                specs = []
                for kt in range(qt + 1):
                    dl = qt - kt
                    bias_ap = BIAS[:, g, dl, :] if dl <= 1 else None
                    specs.append(((i, g, KE[g][:, kt * 128:(kt + 1) * 128], 128, vsl[:, kt, g, :], [B_ksl, B_vsl], 3, kt == 0, kt == qt),
                                  dict(bias_ap=bias_ap, mask_kt=((2 * i + g) % 2, 0 if kt < 32 else 1), sbanks=(0, 1))))
                run_units(specs, skew=1, hooks=hooks)

            def finish_sel(i, g):
                combine(i, g, 3, 1, False)
                if g == 0:
                    return
                yb_t, yb_b = ybt2[i % 2], B_yb2[i % 2]
                tsl = slice(i * 128, (i + 1) * 128)
                P.op('act', lambda e: e.activation(out=ybn[:], in_=yb_t[:].rearrange("p h d -> p (h d)"), func=AF.Square, accum_out=cf[:, 12:13]), reads=[yb_b], writes=[B_ybn, B_cf])
                P.op('act', lambda e: e.activation(out=cf[:, 13:14], in_=cf[:, 12:13], func=AF.Sqrt, scale=1.0 / 512, bias=EPS), reads=[B_cf], writes=[B_cf])
                P.op('dve', lambda e: e.reciprocal(out=cf[:, 13:14], in_=cf[:, 13:14]), reads=[B_cf], writes=[B_cf])
                P.op('dve', lambda e: e.scalar_tensor_tensor(out=ybn[:], in0=yb_t[:].rearrange("p h d -> p (h d)"), scalar=cf[:, 13:14], in1=OGB[:], op0=ALU.mult, op1=ALU.mult),
                     reads=[yb_b, B_cf, B_c2, B_ybn], writes=[B_ybn])
                for cc in range(4):
                    P.op('pe', lambda e, cc=cc: e.transpose(ps[7][:, cc * 128:(cc + 1) * 128], ybn[:, cc * 128:(cc + 1) * 128], ident[:]), reads=[B_ybn, B_const], writes=[psb[7]])
                P.op('act', lambda e: e.activation(out=ybT[:].rearrange("p c q -> p (c q)"), in_=ps[7][:], func=AF.Copy), reads=[psb[7]], writes=[B_ybT])
                P.dma('sp', yn_d[:, 4:8, tsl], ybT[:], reads=[B_ybT], writes=[B_ynd], sem_owner=B_ybT)

            seq = [(i, g) for i in range(n_own) for g in range(2)]
            prev = None
            for (i, g) in seq:
                cw_units(i, g)
                chain_a(i, g)
                if prev is not None:
                    n_units = 4 * prev[0] + 4
                    sel_units(prev[0], prev[1], {n_units // 3: (lambda i=i, g=g: chain_t1(i, g)), (2 * n_units) // 3: (lambda i=i, g=g: chain_t23(i, g))})
                    finish_sel(*prev)
                else:
                    chain_t1(i, g)
                    chain_t23(i, g)
                combine(i, g, 2, 0, True)
                combine(i, g, 4, 2, False)
                prev = (i, g)
            sel_units(prev[0], prev[1], {})
            finish_sel(*prev)
            P.barrier()
            P.emit()
        att.close()

        if stage <= 5:
            return dbg_exit()

        XK = sbuf(es, "p3_XK", [128, 8, 256], BF16)
        XV = sbuf(es, "p3_XV", [128, 2, D], BF16)
        B_XK, B_XV = Buf("XK"), Buf("XV")
        with ExitStack() as pm:
            memt = sbuf(pm, "m_mem", [128, 2, D], F32)
            memn = sbuf(pm, "m_memn", [128, D], F32)
            MN = sbuf(pm, "m_MN", [128, D], F32)
            memnT = sbuf(pm, "m_memnT", [128, 8, 256], BF16)
            wv2 = sbuf(pm, "m_wv2", [128, 8, D], BF16)
            mst = sbuf(pm, "m_st", [128, 4], F32)
            B_mem, B_memn, B_memnT, B_wv2, B_mst = Buf("mem"), Buf("memn"), Buf("memnT"), Buf("wv2"), Buf("mst")
            P.dma('sp', memt[:], mem_d[:, :].rearrange("(t p) f -> p t f", p=128), writes=[B_mem])
            P.dma('sp', MN[:], rowv_d[0:1, R_MEMN:R_MEMN + D].partition_broadcast(128), writes=[B_mem])
            P.dma('pool', wv2[:], xwkv_d[:, D:2 * D].rearrange("(kc p) m -> p kc m", p=128), writes=[B_wv2])
            for mt in range(2):
                P.op('act', lambda e, mt=mt: e.activation(out=memn[:], in_=memt[:, mt, :], func=AF.Square, accum_out=mst[:, 0:1]), reads=[B_mem], writes=[B_memn, B_mst])
                P.op('act', lambda e: e.activation(out=mst[:, 1:2], in_=mst[:, 0:1], func=AF.Sqrt, scale=1.0 / D, bias=EPS), reads=[B_mst], writes=[B_mst])
                P.op('dve', lambda e: e.reciprocal(out=mst[:, 1:2], in_=mst[:, 1:2]), reads=[B_mst], writes=[B_mst])
                P.op('dve', lambda e, mt=mt: e.scalar_tensor_tensor(out=memn[:], in0=memt[:, mt, :], scalar=mst[:, 1:2], in1=MN[:], op0=ALU.mult, op1=ALU.mult),
                     reads=[B_mem, B_mst, B_memn], writes=[B_memn])
                for half in range(2):
                    for cc in range(4):
                        c = half * 4 + cc
                        P.op('pe', lambda e, c=c, cc=cc: e.transpose(ps[7][:, cc * 128:(cc + 1) * 128], memn[:, c * 128:(c + 1) * 128], ident[:]),
                             reads=[B_memn, B_const], writes=[psb[7]])
                    P.op('dve', lambda e, half=half, mt=mt: e.tensor_copy(out=memnT[:, half * 4:half * 4 + 4, mt * 128:(mt + 1) * 128],
                                                                        in_=ps[7][:].rearrange("p (c q) -> p c q", c=4)), reads=[psb[7]], writes=[B_memnT])
            m8s = WStream(pm, "m_w8", 8, 3)
            nxt = m8s.load(xwkv_d, 0)
            for e_ in range(8):
                wt, wb = nxt
                if e_ + 1 < 8:
                    nxt = m8s.load(xwkv_d, e_ + 1)
                pi = e_ % 2
                for k in range(8):
                    P.op('pe', lambda e, k=k, wt=wt, pi=pi: e.matmul(ps[pi][:, 0:256], lhsT=wt[:, k, :], rhs=memnT[:, k, :], start=(k == 0), stop=(k == 7)),
                         reads=[wb, B_memnT], writes=[psb[pi]])
                P.op('act', lambda e, e_=e_, pi=pi: e.activation(out=XK[:, e_, :], in_=ps[pi][:, 0:256], func=AF.Copy), reads=[psb[pi]], writes=[B_XK])
            for mt in range(2):
                for half in range(2):
                    pi = 2 + (mt * 2 + half) % 2
                    for k in range(8):
                        P.op('pe', lambda e, k=k, mt=mt, half=half, pi=pi: e.matmul(ps[pi][:], lhsT=memnT[:, k, mt * 128:(mt + 1) * 128],
                                                                                  rhs=wv2[:, k, half * 512:(half + 1) * 512], start=(k == 0), stop=(k == 7)),
                             reads=[B_wv2, B_memnT], writes=[psb[pi]])
                    P.op('act', lambda e, mt=mt, half=half, pi=pi: e.activation(out=XV[:, mt, half * 512:(half + 1) * 512], in_=ps[pi][:], func=AF.Copy),
                         reads=[psb[pi]], writes=[B_XV])
            P.barrier()
            P.emit()

        with ExitStack() as ph:
            xT = sbuf(ph, "p3_xT", [128, 8, SBN], F32)
            hT = sbuf(ph, "p3_hT", [128, 8, SBN], BF16)
            actT = sbuf(ph, "p3_actT", [128, NJ, SBN], BF16)
            yT = sbuf(ph, "p3_yT", [128, 8, SBN], F32)
            sq = sbuf(ph, "p3_sq", [128, 8, 512], BF16)
            rs = sbuf(ph, "p3_rs", [128, 512], F32)
            sg = [sbuf(ph, "p3_sg%d" % i, [128, 512], F32) for i in range(2)]
            xo = [sbuf(ph, "p3_xo%d" % i, [128, D], F32) for i in range(2)]
            Pm = [sbuf(ph, "p3_Pm%d" % i, [128, 512], BF16) for i in range(2)]
            xb, hb, actb, yb_, rsb = Buf("xT3"), Buf("hT3"), Buf("actT3"), Buf("yT3"), Buf("rs3")
            sqb = [Buf("sq3%d" % i) for i in range(8)]
            sgb = [Buf("sg30"), Buf("sg31")]
            xob = [Buf("xo0"), Buf("xo1")]
            B_Pm = [Buf("Pm0"), Buf("Pm1")]
            wgs = WStream(ph, "p3_wg", 8, 3)
            wus = WStream(ph, "p3_wu", 8, 3)
            wds = WStream(ph, "p3_wd", NJ, 2)
            w8s = WStream(ph, "p3_w8", 8, 3)
            for sbi in range(2):
                tok = slice(sbi * SBN, (sbi + 1) * SBN)
                P.dma('sp', xT[:], x1_d[:, :, tok], reads=[x1db], writes=[xb], parallel=False)
                P.dma('sp', hT[:], yn_d[:, :, tok], reads=[B_ynd], writes=[hb], parallel=False)
                lin_post(ph, hT, hb, 8, wout_d, w8s, xT, xb, SBN, lambda c: gcol[:, G_MIXPOST + c:G_MIXPOST + c + 1], yT, yb_, sq, sqb, rs, rsb)
                rms_fm(xT, xb, G_XAPRE, hT, hb, SBN, sq, sqb, rs, rsb)
                nxt = w8s.load(xwq_d, 0)
                for e_ in range(8):
                    wt, wb = nxt
                    if e_ + 1 < 8:
                        nxt = w8s.load(xwq_d, e_ + 1)
                    for tb in range(2):
                        t0 = tb * 512
                        pi = (e_ * 2 + tb) % 2
                        for k in range(8):
                            P.op('pe', lambda e, k=k, wt=wt, pi=pi, t0=t0: e.matmul(ps[pi][:], lhsT=wt[:, k, :], rhs=hT[:, k, t0:t0 + 512], start=(k == 0), stop=(k == 7)),
                                 reads=[wb, hb], writes=[psb[pi]])
                        P.op('act', lambda e, e_=e_, pi=pi, t0=t0: e.activation(out=actT[:, e_, t0:t0 + 512], in_=ps[pi][:], func=AF.Copy), reads=[psb[pi]], writes=[actb])
                for hh in range(4):
                    for tb in range(2):
                        t0 = tb * 512
                        for mt in range(2):
                            for dc in range(2):
                                P.op('pe', lambda e, hh=hh, mt=mt, dc=dc, t0=t0: e.matmul(ps[mt][:], lhsT=XK[:, 2 * hh + dc, mt * 128:(mt + 1) * 128],
                                                                                        rhs=actT[:, 2 * hh + dc, t0:t0 + 512], start=(dc == 0), stop=(dc == 1)),
                                     reads=[B_XK, actb], writes=[psb[mt]])
                            P.op('act', lambda e, mt=mt: e.activation(out=Pm[mt][:], in_=ps[mt][:], func=AF.Exp, scale=1.0 / 16), reads=[psb[mt]], writes=[B_Pm[mt]])
                        for mt in range(2):
                            P.op('pe', lambda e, mt=mt: e.matmul(ps[6][:], lhsT=ones_bf[:], rhs=Pm[mt][:], start=(mt == 0), stop=(mt == 1)),
                                 reads=[B_Pm[mt], B_const], writes=[psb[6]])
                        for dc in range(2):
                            for mt in range(2):
                                P.op('pe', lambda e, hh=hh, mt=mt, dc=dc: e.matmul(ps[2 + dc][:], lhsT=XV[:, mt, (2 * hh + dc) * 128:(2 * hh + dc + 1) * 128], rhs=Pm[mt][:],
                                                                                 start=(mt == 0), stop=(mt == 1)),
                                     reads=[B_XV, B_Pm[mt]], writes=[psb[2 + dc]])
                        P.op('act', lambda e: e.activation(out=rs[:], in_=ps[6][:], func=AF.Ln), reads=[psb[6]], writes=[rsb])
                        P.op('act', lambda e: e.activation(out=rs[:], in_=rs[:], func=AF.Exp, scale=-1.0), reads=[rsb], writes=[rsb])
                        for dc in range(2):
                            P.op('dve', lambda e, hh=hh, dc=dc, t0=t0: e.tensor_tensor(out=actT[:, 8 + 2 * hh + dc, t0:t0 + 512], in0=ps[2 + dc][:], in1=rs[:], op=ALU.mult),
                                 reads=[psb[2 + dc], rsb, actb], writes=[actb])
                lin_post(ph, actT[:, 8:16, :], actb, 8, xwo_d, w8s, xT, xb, SBN, lambda c: gcol[:, G_XAPOST + c:G_XAPOST + c + 1], yT, yb_, sq, sqb, rs, rsb)
                ffn(ph, 1, xT, xb, hT, hb, actT, actb, yT, yb_, sq, sqb, rs, rsb, sg, sgb, wgs, wus, wds, G_F2PRE, 8)
                for t in range(SBT):
                    oi = t % 2
                    for half in range(2):
                        for cc in range(4):
                            c = half * 4 + cc
                            P.op('pe', lambda e, c=c, cc=cc, t=t: e.transpose(ps[7][:, cc * 128:(cc + 1) * 128], xT[:, c, t * 128:(t + 1) * 128], ident[:]),
                                 reads=[xb, B_const], writes=[psb[7]])
                        P.op('act', lambda e, half=half, oi=oi: e.activation(out=xo[oi][:, half * 512:(half + 1) * 512], in_=ps[7][:], func=AF.Copy),
                             reads=[psb[7]], writes=[xob[oi]])
                    row = (sbi * SBT + t) * 128
                    P.dma('sp', y_d[row:row + 128, :], xo[oi][:], reads=[xob[oi]], writes=[Buf("yout%d" % (sbi * SBT + t))], is_output=True)
            P.finish()
            P.emit()
    return nc


def _t5_bucket(d):
    d = np.maximum(d, 0)
    nf = np.maximum(d, 1).astype(np.float32)
    large = 16 + (np.log(nf / np.float32(16)) / np.float32(math.log(8.0)) * np.float32(16)).astype(np.int32)
    large = np.minimum(large, 31)
    return np.where(d < 16, d, large)


def _consts(r):
    sh_t = 3 - r
    shift = 128 * sh_t
    j0 = 2 * sh_t
    c_first = 8 * sh_t
    ohd = np.zeros((33, 768), np.float32)
    n = np.arange(768)
    d = n - 127
    valid = (d >= 0) & (d < 512)
    bk = _t5_bucket(d)
    ohd[bk[valid], n[valid]] += 1.0
    ohd[31, n[valid]] -= 1.0
    ohd[32, ~valid] = 1.0
    smask = np.zeros((128, NOWN, 2, 128), np.float32)
    jj = np.arange(128)[None, :]
    for i in range(NOWN):
        t = 128 * (4 * i + 3) + np.arange(128)[:, None]
        cur = t // 64
        exists = jj >= j0
        causal = (jj <= cur) & exists
        forced = ((jj == j0) | (jj == cur) | (jj == cur - 1)) & exists
        smask[:, i, 0, :] = (causal & ~forced).astype(np.float32)
        smask[:, i, 1, :] = np.where(forced, 1e4, np.where(causal, 0.0, -1.0))
    kb = np.zeros((128, NT), np.float32)
    pos = 128 * np.arange(NT)[None, :] + np.arange(128)[:, None]
    kb[pos < shift] = NEG
    kbc = np.zeros((128, 4), np.float32)
    cc = 128 * np.arange(4)[None, :] + np.arange(128)[:, None]
    kbc[cc < c_first] = NEG
    kbn = np.zeros((16, NOWN), np.float32)
    cn = (8 * (4 * np.arange(NOWN) + 3) - 9)[None, :] + np.arange(16)[:, None]
    kbn[cn < c_first] = NEG
    def ov(c, j):
        return np.clip(np.minimum(c + 2, 4 * (j + 1)) - np.maximum(c, 4 * j), 0, None).astype(np.float32)
    ovf = ov(cc[:, :, None], np.arange(128)[None, None, :])
    ovn = ov(cn[:, :, None], np.arange(128)[None, None, :])
    return dict(ohd=ohd, smask=smask, kb=kb, kbc=kbc, kbn=kbn, ovf=ovf.astype(np.float32), ovn=ovn.astype(np.float32))


def _col(v):
    return np.ascontiguousarray(np.asarray(v, np.float32).reshape(-1, 128).T)


STAGE = 99
DEBUG = False
_last = {}


def kernel(**inp):
    f = lambda k: np.asarray(inp[k], np.float32)
    x = f('x')
    mem = f('mem')
    w_in = f('w_in')[0]
    qperm = np.arange(512).reshape(2, 4, 64).transpose(1, 0, 2).reshape(-1)
    w_in_r = np.ascontiguousarray(np.concatenate([
        w_in[:, 0:1024], w_in[:, 1024:1536][:, qperm], w_in[:, 1536:1664], w_in[:, 1664:1792], w_in[:, 1792:1920],
        w_in[:, 2048:2176], w_in[:, 1920:2048], w_in[:, 2176:2304], w_in[:, 2304:2328]], axis=1))
    gcol = np.concatenate([_col(f(k)[0]) for k in ('ffn1_pre', 'ffn1_post', 'mix_pre', 'mix_post', 'xa_pre', 'xa_post', 'ffn2_pre', 'ffn2_post')]
                          + [_col(f('out_gain_a')[0]), _col(f('ck_b1')[0]), _col(f('cv_b1')[0])], axis=1)
    peT = np.ascontiguousarray(np.concatenate([f('ck_pe')[0].T, f('cv_pe')[0].T], axis=1))
    rowv = np.concatenate([f('gm_ln_g')[0].reshape(-1), f('gm_ln_b')[0].reshape(-1), f('out_gain_b')[0].reshape(-1),
                           f('mem_norm')[0].reshape(-1), f('gm_bs')[0].reshape(-1)])[None, :]
    shared = dict(
        wg1=f('ffn1_wg')[0], wu1=f('ffn1_wu')[0], wd1=f('ffn1_wd')[0], wg2=f('ffn2_wg')[0], wu2=f('ffn2_wu')[0], wd2=f('ffn2_wd')[0],
        w_in=w_in_r, w_out=f('w_out')[0], gm_ws=f('gm_ws')[0], ck_w1=f('ck_w1')[0], cv_w1=f('cv_w1')[0], ck_w2=f('ck_w2')[0], cv_w2=f('cv_w2')[0],
        xa_wq=f('xa_wq')[0], xa_wkv=f('xa_wkv')[0], xa_wo=f('xa_wo')[0], gcol=np.ascontiguousarray(gcol), peT=peT,
        rowv=np.ascontiguousarray(rowv), rel_bias=f('rel_bias'))
    consts = [_consts(r) for r in range(4)]
    in_maps = []
    for c in range(8):
        b, r = c // 4, c % 4
        shift = 128 * (3 - r)
        xc = np.zeros((SEQ, D), np.float32)
        xc[shift:] = x[b, :SEQ - shift]
        m = dict(shared)
        m.update(consts[r])
        m['x_ctx'] = xc
        m['mem'] = np.ascontiguousarray(mem[b])
        in_maps.append(m)
    nc = build(stage=STAGE, debug=DEBUG)
    res = run_bass_kernel_spmd(nc, in_maps, core_ids=list(range(8)))
    _last['res'] = res
    out = np.zeros((2, SEQ, D), np.float32)
    for c in range(8):
        b, r = c // 4, c % 4
        y = res.results[c]["y_own"].reshape(NOWN, 128, D)
        for i in range(NOWN):
            qt = 4 * i + r
            out[b, qt * 128:(qt + 1) * 128] = y[i]
    return out
```

```python
import math
from contextlib import ExitStack

import numpy as np
import concourse.bass as bass
import concourse.mybir as mybir
from concourse.bass_utils import run_bass_kernel_spmd

F32 = mybir.dt.float32
BF16 = mybir.dt.bfloat16
AF = mybir.ActivationFunctionType
ALU = mybir.AluOpType
AX = mybir.AxisListType
ENG = ('pe', 'act', 'dve', 'pool', 'sp')

D = 1024
DFF = 2816
NJ = DFF // 128
SEQ = 8192
NT = 64
NOWN = 16
SBT = 8
SBN = SBT * 128
EPS = 1e-6
NEG = -30000.0
BIGNEG = 32768.0
SCALE = 0.125

C_U, C_V, C_Q, C_KCR, C_VCR, C_KSL, C_KWN, C_VSL, C_VWN, C_GT = 0, 512, 1024, 1536, 1664, 1792, 1920, 2048, 2176, 2304
G_F1PRE, G_F1POST, G_MIXPRE, G_MIXPOST, G_XAPRE, G_XAPOST, G_F2PRE, G_F2POST = [8 * i for i in range(8)]
G_OGA, G_CKB1, G_CVB1, NGCOL = 64, 68, 70, 72
R_LNG, R_LNB, R_OGB, R_MEMN, R_BS, NROW = 0, 512, 1024, 1536, 2560, 3072


class Buf:
    __slots__ = ('name', 'w', 'r', 'dsem', 'dcnt')

    def __init__(self, name):
        self.name = name
        self.w = None
        self.r = {}
        self.dsem = None
        self.dcnt = 0


class Prog:
    def __init__(self, nc, es):
        self.nc = nc
        self.es = es
        self.q = {k: [] for k in ENG}
        self.sem = {k: es.enter_context(nc.semaphore('s_' + k)) for k in ENG}
        self.cnt = {k: 0 for k in ENG}
        self.known = {k: {} for k in ENG}
        self.dbufs = []
        self.out_waits = []

    def _deps(self, reads, writes, skip_waw=()):
        d = {}
        for b in reads:
            if b.w is not None and d.get(b.w[0], 0) < b.w[1]:
                d[b.w[0]] = b.w[1]
        for b in writes:
            if b.w is not None and b not in skip_waw and d.get(b.w[0], 0) < b.w[1]:
                d[b.w[0]] = b.w[1]
            for sem, v in b.r.items():
                if d.get(sem, 0) < v:
                    d[sem] = v
        return d

    def _waits(self, eng, d):
        waits = []
        kn = self.known[eng]
        for sem, v in d.items():
            if eng == 'pe' and sem is self.sem['pe']:
                continue
            if kn.get(sem, 0) < v:
                waits.append((sem, v))
                kn[sem] = v
        return waits

    def op(self, eng, fn, reads=(), writes=()):
        waits = self._waits(eng, self._deps(reads, writes))
        self.cnt[eng] += 1
        n = self.cnt[eng]
        mysem = self.sem[eng]

        def thunk(e):
            for sem, v in waits:
                e.wait_ge(sem, v)
            fn(e).then_inc(mysem, 1)
        self.q[eng].append(thunk)
        for b in writes:
            b.w = (mysem, n)
            b.r = {}
        for b in reads:
            b.r[mysem] = n

    def dma(self, eng, out_ap, in_ap, reads=(), writes=(), parallel=True, is_output=False, sem_owner=None):
        wb = sem_owner if sem_owner is not None else writes[0]
        if wb.dsem is None:
            wb.dsem = self.es.enter_context(self.nc.semaphore('d_' + wb.name))
            self.dbufs.append(wb)
        skip = tuple(b for b in writes if parallel and b.w is not None and (b.w[0] is b.dsem or sem_owner is not None))
        waits = self._waits(eng, self._deps(reads, writes, skip_waw=skip))
        wb.dcnt += 16
        v = wb.dcnt
        sem = wb.dsem

        def thunk(e):
            for s_, v_ in waits:
                e.wait_ge(s_, v_)
            e.dma_start(out=out_ap, in_=in_ap).then_inc(sem, 16)
        self.q[eng].append(thunk)
        for b in writes:
            b.w = (sem, v)
            b.r = {}
        for b in reads:
            b.r[sem] = v
        if is_output:
            self.out_waits.append((sem, v))

    def barrier(self):
        d = {self.sem[k]: self.cnt[k] for k in ENG if self.cnt[k] > 0}
        for b in self.dbufs:
            d[b.dsem] = b.dcnt
        for k in ENG:
            waits = self._waits(k, dict(d))
            if waits:
                self.q[k].append(lambda e, waits=waits: [e.wait_ge(s_, v_) for s_, v_ in waits])

    def finish(self):
        d = {}
        for sem, v in self.out_waits:
            if d.get(sem, 0) < v:
                d[sem] = v
        waits = list(d.items())
        self.q['sp'].append(lambda e: [e.wait_ge(s_, v_) for s_, v_ in waits])

    def emit(self):
        nc = self.nc
        q = self.q
        with nc.Block() as block:
            @block.tensor
            def _(e):
                for f in q['pe']:
                    f(e)

            @block.scalar
            def _(e):
                for f in q['act']:
                    f(e)

            @block.vector
            def _(e):
                for f in q['dve']:
                    f(e)

            @block.gpsimd
            def _(e):
                for f in q['pool']:
                    f(e)

            @block.sync
            def _(e):
                for f in q['sp']:
                    f(e)
        self.q = {k: [] for k in ENG}


def build(stage=99, debug=False):
    nc = bass.Bass("TRN2", target_bir_lowering=False)

    def din(name, shape):
        return nc.dram_tensor(name, list(shape), F32, kind="ExternalInput")

    x_d = din("x_ctx", [SEQ, D])
    mem_d = din("mem", [256, D])
    wg_d = [din("wg1", [D, DFF]), din("wg2", [D, DFF])]
    wu_d = [din("wu1", [D, DFF]), din("wu2", [D, DFF])]
    wd_d = [din("wd1", [DFF, D]), din("wd2", [DFF, D])]
    win_d = din("w_in", [D, 2328])
    wout_d = din("w_out", [D, D])
    ws_d = din("gm_ws", [4, 128, 128])
    cw1_d = [din("ck_w1", [2048, 256]), din("cv_w1", [2048, 256])]
    cw2_d = [din("ck_w2", [256, 64]), din("cv_w2", [256, 64])]
    xwq_d = din("xa_wq", [D, D])
    xwkv_d = din("xa_wkv", [D, 2 * D])
    xwo_d = din("xa_wo", [D, D])
    gcol_d = din("gcol", [128, NGCOL])
    peT_d = din("peT", [64, 64])
    rowv_d = din("rowv", [1, NROW])
    tab_d = din("rel_bias", [32, 8])
    ohd_d = din("ohd", [33, 768])
    smask_d = din("smask", [128, NOWN, 2, 128])
    kb_d = din("kb", [128, NT])
    kbc_d = din("kbc", [128, 4])
    kbn_d = din("kbn", [16, NOWN])
    ovf_d = din("ovf", [128, 4, 128])
    ovn_d = din("ovn", [16, NOWN, 128])
    y_d = nc.dram_tensor("y_own", [NOWN * 128, D], F32, kind="ExternalOutput")

    skind = "ExternalOutput" if debug else "Internal"
    kt_d = [nc.dram_tensor("sc_kt%d" % s, [128, SEQ], BF16, kind=skind) for s in range(4)]
    vt_d = nc.dram_tensor("sc_vt", [SEQ, 256], BF16, kind=skind)
    x1_d = nc.dram_tensor("sc_x1", [128, 8, NOWN * 128], F32, kind=skind)
    fv_d = nc.dram_tensor("sc_fv", [8, 768], F32, kind="Internal")

    with ExitStack() as es:
        P = Prog(nc, es)

        def sbuf(st, name, shape, dt):
            return st.enter_context(nc.sbuf_tensor("sb_" + name, list(shape), dt))

        ps = [es.enter_context(nc.psum_tensor("ps%d" % i, [128, 512], F32)) for i in range(8)]
        psb = [Buf("ps%d" % i) for i in range(8)]

        ones_bf = sbuf(es, "ones_bf", [128, 128], BF16)
        ones_f = sbuf(es, "ones_f", [128, 128], F32)
        ident = sbuf(es, "ident", [128, 128], F32)
        antid = sbuf(es, "antid", [128, 128], F32)
        gcol = sbuf(es, "gcol", [128, NGCOL], F32)
        ghalf = sbuf(es, "ghalf", [128, 16], F32)
        zcol = sbuf(es, "zcol", [128, 1], F32)
        B_const = Buf("const")
        B_ghalf = Buf("ghalf")
        P.op('pool', lambda e: e.memset(ones_f[:], 1.0), writes=[B_const])
        P.op('pool', lambda e: e.memset(ones_bf[:], 1.0), writes=[B_const])
        P.op('pool', lambda e: e.memset(zcol[:], 0.0), writes=[B_const])
        P.op('pool', lambda e: e.affine_select(out=ident[:], in_=ones_f[:], pattern=[[-1, 128]], compare_op=ALU.is_equal,
                                               fill=0.0, base=0, channel_multiplier=1), reads=[B_const], writes=[B_const])
        P.op('pool', lambda e: e.affine_select(out=antid[:], in_=ones_f[:], pattern=[[1, 128]], compare_op=ALU.is_equal,
                                               fill=0.0, base=-127, channel_multiplier=1), reads=[B_const], writes=[B_const])
        B_gcol = Buf("gcol")
        P.dma('sp', gcol[:], gcol_d[:, :], writes=[B_gcol])
        P.op('dve', lambda e: e.tensor_scalar(out=ghalf[:, 0:8], in0=gcol[:, G_F1POST:G_F1POST + 8], scalar1=0.5, scalar2=None, op0=ALU.mult),
             reads=[B_gcol], writes=[B_ghalf])
        P.op('dve', lambda e: e.tensor_scalar(out=ghalf[:, 8:16], in0=gcol[:, G_F2POST:G_F2POST + 8], scalar1=0.5, scalar2=None, op0=ALU.mult),
             reads=[B_gcol], writes=[B_ghalf])

        def bsel(b, tb):
            return b[tb] if isinstance(b, (list, tuple)) else b

        def rstd_from_stat(st, stat_ps, stat_b, rs_t, rs_b, n, width, rows=128):
            P.op('act', lambda e: e.activation(out=rs_t[:rows, :width], in_=stat_ps[:rows, :width], func=AF.Ln, scale=1.0 / n, bias=EPS),
                 reads=[stat_b], writes=[rs_b])
            P.op('act', lambda e: e.activation(out=rs_t[:rows, :width], in_=rs_t[:rows, :width], func=AF.Exp, scale=-0.5), reads=[rs_b], writes=[rs_b])

        def rms_fm(xT, xb, gofs, hT, hb, ntok, sq, sqb, rs, rsb, stat_i=6, t_start=0):
            for t0 in range(t_start, t_start + ntok, 512):
                w = min(512, t_start + ntok - t0)
                tb = t0 // 512
                xb_, hb_, rs_, rsb_ = bsel(xb, tb), bsel(hb, tb), bsel(rs, tb), bsel(rsb, tb)
                for c in range(8):
                    P.op('act', lambda e, c=c, t0=t0, w=w: e.activation(out=sq[:, c, :w], in_=xT[:, c, t0:t0 + w], func=AF.Square),
                         reads=[xb_], writes=[sqb[c]])
                    P.op('pe', lambda e, c=c, w=w: e.matmul(ps[stat_i][:, :w], lhsT=ones_bf[:], rhs=sq[:, c, :w], start=(c == 0), stop=(c == 7)),
                         reads=[sqb[c], B_const], writes=[psb[stat_i]])
                rstd_from_stat(None, ps[stat_i], psb[stat_i], rs_, rsb_, float(D), w)
                for c in range(8):
                    P.op('dve', lambda e, c=c, t0=t0, w=w, rs_=rs_: e.scalar_tensor_tensor(out=hT[:, c, t0:t0 + w], in0=xT[:, c, t0:t0 + w],
                                                                                         scalar=gcol[:, gofs + c:gofs + c + 1], in1=rs_[:, :w],
                                                                                         op0=ALU.mult, op1=ALU.mult),
                         reads=[xb_, rsb_, B_gcol], writes=[hb_])

        class WStream:
            def __init__(self, st, name, kc, nbuf):
                self.kc = kc
                self.t = [sbuf(st, "%s%d" % (name, i), [128, kc, 128], BF16) for i in range(nbuf)]
                self.b = [Buf("%s%d" % (name, i)) for i in range(nbuf)]
                self.n = 0

            def load(self, w_dram, c):
                i = self.n % len(self.t)
                self.n += 1
                src = w_dram[:, c * 128:(c + 1) * 128].rearrange("(kc p) m -> p kc m", p=128)
                P.dma('pool', self.t[i][:], src, writes=[self.b[i]], parallel=False)
                return self.t[i], self.b[i]

        def lin_post(st, src, srcb, KC, w_dram, wstream, xT, xb, ntok, gain_ap_fn, yT, yb_, sq, sqb, rs, rsb, after_block=None):
            nb = (ntok + 511) // 512
            pend_stat = None
            nxt = wstream.load(w_dram, 0)
            for c in range(8):
                wt, wb = nxt
                if c + 1 < 8:
                    nxt = wstream.load(w_dram, c + 1)
                for tb in range(nb):
                    t0 = tb * 512
                    w = min(512, ntok - t0)
                    yi = 4 + (c * nb + tb) % 2
                    for k in range(KC):
                        P.op('pe', lambda e, k=k, t0=t0, w=w, wt=wt, yi=yi: e.matmul(ps[yi][:, :w], lhsT=wt[:, k, :], rhs=src[:, k, t0:t0 + w],
                                                                                   start=(k == 0), stop=(k == KC - 1)),
                             reads=[wb, bsel(srcb, tb)], writes=[psb[yi]])
                    P.op('act', lambda e, c=c, t0=t0, w=w, yi=yi: e.activation(out=yT[:, c, t0:t0 + w], in_=ps[yi][:, :w], func=AF.Copy),
                         reads=[psb[yi]], writes=[bsel(yb_, tb)])
                    P.op('act', lambda e, c=c, w=w, yi=yi, tb=tb: e.activation(out=sq[:, (c * nb + tb) % 8, :w], in_=ps[yi][:, :w], func=AF.Square),
                         reads=[psb[yi]], writes=[sqb[(c * nb + tb) % 8]])
                    if pend_stat is not None:
                        pend_stat()
                    pend_stat = (lambda c=c, w=w, tb=tb: P.op(
                        'pe', lambda e: e.matmul(ps[6 + tb][:, :w], lhsT=ones_bf[:], rhs=sq[:, (c * nb + tb) % 8, :w], start=(c == 0), stop=(c == 7)),
                        reads=[sqb[(c * nb + tb) % 8], B_const], writes=[psb[6 + tb]]))
            if pend_stat is not None:
                pend_stat()
            for tb in range(nb):
                t0 = tb * 512
                w = min(512, ntok - t0)
                rs_, rsb_, ybb, xbb = bsel(rs, tb), bsel(rsb, tb), bsel(yb_, tb), bsel(xb, tb)
                rstd_from_stat(None, ps[6 + tb], psb[6 + tb], rs_, rsb_, float(D), w)
                for c in range(8):
                    P.op('dve', lambda e, c=c, t0=t0, w=w, rs_=rs_: e.scalar_tensor_tensor(out=yT[:, c, t0:t0 + w], in0=yT[:, c, t0:t0 + w],
                                                                                         scalar=gain_ap_fn(c), in1=rs_[:, :w], op0=ALU.mult, op1=ALU.mult),
                         reads=[ybb, rsb_, B_gcol, B_ghalf], writes=[ybb])
                    P.op('dve', lambda e, c=c, t0=t0, w=w: e.tensor_tensor(out=xT[:, c, t0:t0 + w], in0=xT[:, c, t0:t0 + w], in1=yT[:, c, t0:t0 + w], op=ALU.add),
                         reads=[ybb, xbb], writes=[xbb])
                if after_block is not None:
                    after_block(tb)

        def ffn(st, li, xT, xb, hT, hb, actT, actb, yT, yb_, sq, sqb, rs, rsb, sg, sgb, wgs, wus, wds, gpre, ghalf_ofs, after_block=None):
            rms_fm(xT, xb, gpre, hT, hb, SBN, sq, sqb, rs, rsb)
            nxt = (wgs.load(wg_d[li], 0), wus.load(wu_d[li], 0))
            for j in range(NJ):
                (gt_, gb_), (ut_, ub_) = nxt
                if j + 1 < NJ:
                    nxt = (wgs.load(wg_d[li], j + 1), wus.load(wu_d[li], j + 1))
                for tb in range(2):
                    t0 = tb * 512
                    gi = (j * 2 + tb) % 2
                    ui = 2 + gi
                    for k in range(8):
                        P.op('pe', lambda e, k=k, t0=t0, gt_=gt_, gi=gi: e.matmul(ps[gi][:], lhsT=gt_[:, k, :], rhs=hT[:, k, t0:t0 + 512],
                                                                                start=(k == 0), stop=(k == 7)),
                             reads=[gb_, bsel(hb, tb)], writes=[psb[gi]])
                    for k in range(8):
                        P.op('pe', lambda e, k=k, t0=t0, ut_=ut_, ui=ui: e.matmul(ps[ui][:], lhsT=ut_[:, k, :], rhs=hT[:, k, t0:t0 + 512],
                                                                                start=(k == 0), stop=(k == 7)),
                             reads=[ub_, bsel(hb, tb)], writes=[psb[ui]])
                    P.op('act', lambda e, gi=gi: e.activation(out=sg[gi][:], in_=ps[gi][:], func=AF.Silu), reads=[psb[gi]], writes=[sgb[gi]])
                    P.op('dve', lambda e, gi=gi, ui=ui, j=j, t0=t0: e.tensor_tensor(out=actT[:, j, t0:t0 + 512], in0=sg[gi][:], in1=ps[ui][:], op=ALU.mult),
                         reads=[sgb[gi], psb[ui]], writes=[bsel(actb, tb)])
            lin_post(st, actT, actb, NJ, wd_d[li], wds, xT, xb, SBN, lambda c: ghalf[:, ghalf_ofs + c:ghalf_ofs + c + 1], yT, yb_, sq, sqb, rs, rsb,
                     after_block=after_block)

        n_sb = NT // SBT if stage >= 2 else 1
        with ExitStack() as ph:
            xT = sbuf(ph, "p1_xT", [128, 8, SBN], F32)
            hT = sbuf(ph, "p1_hT", [128, 8, SBN], BF16)
            actT = sbuf(ph, "p1_actT", [128, NJ, SBN], BF16)
            yT = sbuf(ph, "p1_yT", [128, 8, SBN], F32)
            sq = sbuf(ph, "p1_sq", [128, 8, 512], BF16)
            rs = sbuf(ph, "p1_rs", [128, 512], F32)
            sg = [sbuf(ph, "p1_sg%d" % i, [128, 512], F32) for i in range(2)]
            xin = [sbuf(ph, "p1_xin%d" % i, [128, D], F32) for i in range(4)]
            wkf = sbuf(ph, "p1_wkf", [128, 8, 512], BF16)
            wvt = sbuf(ph, "p1_wvt", [128, 8, 256], BF16)
            kst = sbuf(ph, "p1_kst", [128, 4, SBN], BF16)
            vst = sbuf(ph, "p1_vst", [128, SBT, 256], BF16)
            xb, hb, actb, yb_, rsb = [[Buf(n + "0"), Buf(n + "1")] for n in ("xT", "hT", "actT", "yT", "rs")]
            rs = [rs, sbuf(ph, "p1_rs1", [128, 512], F32)]
            sqb = [Buf("sq%d" % i) for i in range(8)]
            sgb = [Buf("sg0"), Buf("sg1")]
            xinb = [Buf("xin%d" % i) for i in range(4)]
            B_wk = Buf("wkf")
            kstb = [[Buf("kst%d%d" % (a_, b_)) for b_ in range(4)] for a_ in range(2)]
            vstb = [Buf("vst0"), Buf("vst1")]
            xstb = [Buf("xst0"), Buf("xst1")]
            ktdb = [Buf("ktd%d" % s) for s in range(4)]
            vtdb, x1db = Buf("vtd"), Buf("x1d")
            wgs = WStream(ph, "p1_wg", 8, 3)
            wus = WStream(ph, "p1_wu", 8, 3)
            wds = WStream(ph, "p1_wd", NJ, 2)
            P.dma('pool', wkf[:], win_d[:, C_KCR:C_KCR + 512].rearrange("(kc p) m -> p kc m", p=128), writes=[B_wk])
            P.dma('pool', wvt[:], win_d[:, C_VSL:C_VSL + 256].rearrange("(kc p) m -> p kc m", p=128), writes=[B_wk])

            for sbi in range(n_sb):
                for t in range(SBT):
                    gt = sbi * SBT + t
                    xi = gt % 4
                    P.dma('sp', xin[xi][:], x_d[gt * 128:(gt + 1) * 128, :], writes=[xinb[xi]], parallel=False)
                    for half in range(2):
                        for cc in range(4):
                            c = half * 4 + cc
                            P.op('pe', lambda e, xi=xi, c=c, cc=cc: e.transpose(ps[7][:, cc * 128:(cc + 1) * 128], xin[xi][:, c * 128:(c + 1) * 128], ident[:]),
                                 reads=[xinb[xi], B_const], writes=[psb[7]])
                        P.op('dve', lambda e, half=half, t=t: e.tensor_copy(out=xT[:, half * 4:half * 4 + 4, t * 128:(t + 1) * 128],
                                                                          in_=ps[7][:].rearrange("p (c q) -> p c q", c=4)),
                             reads=[psb[7]], writes=[xb[t // 4]])
                def tail(tb, sbi=sbi):
                    t0 = tb * 512
                    for t in (3, 7):
                        if t // 4 == tb:
                            i_own = sbi * 2 + (1 if t == 7 else 0)
                            P.dma('pool', x1_d[:, :, i_own * 128:(i_own + 1) * 128], xT[:, :, t * 128:(t + 1) * 128], reads=[xb[tb]], writes=[x1db], sem_owner=xstb[tb])
                    rms_fm(xT, xb, G_MIXPRE, hT, hb, 512, sq, sqb, rs, rsb, t_start=t0)
                    for s_ in range(4):
                        pi = s_ % 2
                        for k in range(8):
                            P.op('pe', lambda e, k=k, s_=s_, pi=pi: e.matmul(ps[pi][:], lhsT=wkf[:, k, s_ * 128:(s_ + 1) * 128], rhs=hT[:, k, t0:t0 + 512],
                                                                         start=(k == 0), stop=(k == 7)),
                                 reads=[B_wk, hb[tb]], writes=[psb[pi]])
                        P.op('act', lambda e, s_=s_, pi=pi: e.activation(out=kst[:, s_, t0:t0 + 512], in_=ps[pi][:], func=AF.Copy),
                             reads=[psb[pi]], writes=[kstb[tb][s_]])
                        P.dma('pool', kt_d[s_][:, sbi * SBN + t0:sbi * SBN + t0 + 512], kst[:, s_, t0:t0 + 512], reads=[kstb[tb][s_]], writes=[ktdb[s_]],
                              sem_owner=kstb[tb][s_])
                    for t in range(tb * 4, tb * 4 + 4):
                        pi = 2 + t % 2
                        for k in range(8):
                            P.op('pe', lambda e, k=k, t=t, pi=pi: e.matmul(ps[pi][:, 0:256], lhsT=hT[:, k, t * 128:(t + 1) * 128], rhs=wvt[:, k, :],
                                                                         start=(k == 0), stop=(k == 7)),
                                 reads=[B_wk, hb[tb]], writes=[psb[pi]])
                        P.op('dve', lambda e, t=t, pi=pi: e.tensor_copy(out=vst[:, t, :], in_=ps[pi][:, 0:256]), reads=[psb[pi]], writes=[vstb[tb]])
                    r0 = sbi * SBN + t0
                    P.dma('pool', vt_d[r0:r0 + 512, :].rearrange("(t p) m -> p t m", p=128), vst[:, tb * 4:tb * 4 + 4, :], reads=[vstb[tb]], writes=[vtdb], sem_owner=vstb[tb])

                ffn(ph, 0, xT, xb, hT, hb, actT, actb, yT, yb_, sq, sqb, rs, rsb, sg, sgb, wgs, wus, wds, G_F1PRE, 0, after_block=tail)
            P.barrier()
            P.emit()

        def dbg_exit():
            with ExitStack() as phd:
                tmp = sbuf(phd, "dbg_t", [128, D], F32)
                tb_ = Buf("dbg_t")
                P.op('pool', lambda e: e.memset(tmp[:], 0.0), writes=[tb_])
                for i in range(NOWN):
                    P.dma('sp', y_d[i * 128:(i + 1) * 128, :], tmp[:], reads=[tb_], writes=[Buf("yo%d" % i)], is_output=True)
                P.finish()
                P.emit()
            return nc

        if stage <= 2:
            return dbg_exit()

        att = es.enter_context(ExitStack())
        KcT = sbuf(att, "KcT", [128, 512], BF16)
        Vc = sbuf(att, "Vc", [128, 4, 2, 65], BF16)
        VcN = sbuf(att, "VcN", [16, NOWN, 2, 65], BF16)
        KE = [sbuf(att, "KE%d" % g_, [128, SEQ], BF16) for g_ in range(2)]
        kwnT = sbuf(att, "kwnT", [128, SEQ], BF16)
        vsl = sbuf(att, "vsl", [128, NT, 2, 65], BF16)
        vwn = sbuf(att, "vwn", [128, NT, 2, 65], BF16)
        QT = sbuf(att, "QT", [128, 4, NOWN * 128], BF16)
        sig = sbuf(att, "sig", [128, NOWN, 24], F32)
        B_KcT, B_Vc, B_VcN, B_ksl, B_kwn, B_vsl, B_vwn, B_QT, B_sig = [Buf(n) for n in
                                                                      ("KcT", "Vc", "VcN", "kslT", "kwnT", "vsl", "vwn", "QT", "sig")]
        yn_d = nc.dram_tensor("sc_yn", [128, 8, NOWN * 128], BF16, kind=skind)
        B_ynd = Buf("ynd")
        with ExitStack() as ph:
            rawT = [sbuf(ph, "c_raw%d" % kv, [128, SEQ], BF16) for kv in range(2)]
            w1 = [sbuf(ph, "c_w1%d" % kv, [128, 32, 256], BF16) for kv in range(2)]
            w2x = [sbuf(ph, "c_w2%d" % kv, [128, 2, 128], BF16) for kv in range(2)]
            hid = [sbuf(ph, "c_hid%d" % kv, [128, 2, 2, 512], BF16) for kv in range(2)]
            pe_bf = sbuf(ph, "c_pe", [64, 64], BF16)
            cvec = sbuf(ph, "c_cvec", [128, 2, 2], F32)
            B_raw = [Buf("raw0"), Buf("raw1")]
            B_w1 = [Buf("w10"), Buf("w11")]
            B_hid = [Buf("hid0"), Buf("hid1")]
            B_cv = Buf("cvec")
            B_pe = Buf("pe")
            P.dma('pool', pe_bf[:], peT_d[:, :], writes=[B_pe])
            for kv in range(2):
                P.dma('sp', rawT[kv][:], kt_d[kv][:, :], reads=[ktdb[kv]], writes=[B_raw[kv]])
                for hh in range(2):
                    P.dma('pool', w1[kv][hh * 64:(hh + 1) * 64, :, :], cw1_d[kv][:, :].rearrange("(p d) j -> d p j", d=64), writes=[B_w1[kv]])
                    P.dma('pool', w2x[kv][:, :, hh * 64:(hh + 1) * 64], cw2_d[kv][:, :].rearrange("(jc p) d -> p jc d", p=128), writes=[B_w1[kv]])
                P.op('pool', lambda e, kv=kv: e.memset(hid[kv][:], 0.0), writes=[B_hid[kv]])
            P.op('pool', lambda e: e.memset(Vc[:, :, :, 64:65], 1.0), writes=[B_Vc])
            P.op('pool', lambda e: e.memset(VcN[:, :, :, 64:65], 1.0), writes=[B_VcN])
            for g_ in range(2):
                oth = slice(64, 128) if g_ == 0 else slice(0, 64)
                P.op('pool', lambda e, g_=g_, oth=oth: e.memset(KE[g_][oth, :], 1.0), writes=[B_ksl])
                for cmp_base, pat, cm in ((0, [[0, 2], [128, 32], [1, 128]], -64), (63, [[0, 2], [-128, 32], [-1, 128]], 64)):
                    P.op('pool', lambda e, g_=g_, oth=oth, cmp_base=cmp_base, pat=pat, cm=cm: e.affine_select(
                        out=KE[g_][oth, :].rearrange("p (a b k) -> p a b k", a=2, b=32), in_=KE[g_][oth, :].rearrange("p (a b k) -> p a b k", a=2, b=32),
                        pattern=pat, compare_op=ALU.is_ge, fill=0.0, base=cmp_base, channel_multiplier=cm), reads=[B_ksl], writes=[B_ksl])
                P.dma('sp', KE[g_][64 * g_:64 * g_ + 64, :], kt_d[2][64 * g_:64 * g_ + 64, :], reads=[ktdb[2], B_ksl], writes=[B_ksl])
            P.dma('sp', kwnT[:], kt_d[3][:, :], reads=[ktdb[3]], writes=[B_kwn])
            P.op('pool', lambda e: e.memset(vsl[:, :, :, 64:65], 1.0), writes=[B_vsl])
            P.op('pool', lambda e: e.memset(vwn[:, :, :, 64:65], 1.0), writes=[B_vwn])
            for g in range(2):
                P.dma('sp', vsl[:, :, g, 0:64], vt_d[:, g * 64:(g + 1) * 64].rearrange("(t p) d -> p t d", p=128), reads=[vtdb, B_vsl], writes=[B_vsl])
                P.dma('sp', vwn[:, :, g, 0:64], vt_d[:, 128 + g * 64:128 + (g + 1) * 64].rearrange("(t p) d -> p t d", p=128), reads=[vtdb, B_vwn], writes=[B_vwn])

            for kv in range(2):
                for jc in range(2):
                    for p_ in range(32):
                        P.op('pe', lambda e, kv=kv, jc=jc, p_=p_: e.matmul(ps[0][:, jc:jc + 1], lhsT=w1[kv][0:64, p_, jc * 128:(jc + 1) * 128],
                                                                         rhs=pe_bf[0:64, kv * 32 + p_:kv * 32 + p_ + 1], start=(p_ == 0), stop=(p_ == 31)),
                             reads=[B_w1[kv], B_pe], writes=[psb[0]])
                gofs = G_CKB1 if kv == 0 else G_CVB1
                P.op('dve', lambda e, kv=kv, gofs=gofs: e.tensor_tensor(out=cvec[:, kv, :], in0=ps[0][:, 0:2], in1=gcol[:, gofs:gofs + 2], op=ALU.add),
                     reads=[psb[0], B_gcol], writes=[B_cv])
                r3 = rawT[kv][:, :].rearrange("q (c s) -> q c s", s=16)
                for g in range(2):
                    for jc in range(2):
                        hi = 1 + (g * 2 + jc) % 2
                        for p_ in range(32):
                            rhs = r3[64 * g:64 * g + 64, 0:511, p_] if p_ < 16 else r3[64 * g:64 * g + 64, 1:512, p_ - 16]
                            P.op('pe', lambda e, kv=kv, g=g, jc=jc, p_=p_, rhs=rhs, hi=hi: e.matmul(
                                ps[hi][:, 0:511], lhsT=w1[kv][64 * g:64 * g + 64, p_, jc * 128:(jc + 1) * 128], rhs=rhs, start=(p_ == 0), stop=(p_ == 31)),
                                reads=[B_w1[kv], B_raw[kv]], writes=[psb[hi]])
                        P.op('act', lambda e, kv=kv, g=g, jc=jc, hi=hi: e.activation(out=hid[kv][:, jc, g, 0:511], in_=ps[hi][:, 0:511], func=AF.Gelu,
                                                                                 bias=cvec[:, kv, jc:jc + 1]),
                             reads=[psb[hi], B_cv], writes=[B_hid[kv]])
            for g in range(2):
                for jc in range(2):
                    P.op('pe', lambda e, g=g, jc=jc: e.matmul(ps[3][:, 0:511], lhsT=w2x[0][:, jc, :], rhs=hid[0][:, jc, g, 0:511], start=(jc == 0), stop=(jc == 1)),
                         reads=[B_w1[0], B_hid[0]], writes=[psb[3]])
                P.op('act', lambda e, g=g: e.activation(out=KcT[64 * g:64 * g + 64, 0:511], in_=ps[3][64 * g:64 * g + 64, 0:511], func=AF.Copy),
                     reads=[psb[3]], writes=[B_KcT])
                for ct in range(4):
                    for jc in range(2):
                        P.op('pe', lambda e, g=g, jc=jc, ct=ct: e.matmul(ps[4][:, 0:64], lhsT=hid[1][:, jc, g, ct * 128:(ct + 1) * 128], rhs=w2x[1][:, jc, 0:64],
                                                                       start=(jc == 0), stop=(jc == 1)),
                             reads=[B_w1[1], B_hid[1]], writes=[psb[4]])
                    P.op('dve', lambda e, g=g, ct=ct: e.tensor_copy(out=Vc[:, ct, g, 0:64], in_=ps[4][:, 0:64]), reads=[psb[4]], writes=[B_Vc])
                for i in range(NOWN):
                    c0 = 32 * i + 15
                    for jc in range(2):
                        P.op('pe', lambda e, g=g, jc=jc, c0=c0: e.matmul(ps[5][0:16, 0:64], lhsT=hid[1][:, jc, g, c0:c0 + 16], rhs=w2x[1][:, jc, 0:64],
                                                                       start=(jc == 0), stop=(jc == 1)),
                             reads=[B_w1[1], B_hid[1]], writes=[psb[5]])
                    P.op('dve', lambda e, g=g, i=i: e.tensor_copy(out=VcN[0:16, i, g, 0:64], in_=ps[5][0:16, 0:64]), reads=[psb[5]], writes=[B_VcN])
            P.barrier()
            P.emit()

        with ExitStack() as ph:
            wu_sb = sbuf(ph, "a_wu", [128, 8, 512], BF16)
            wv_sb = sbuf(ph, "a_wv", [128, 8, 512], BF16)
            wq_sb = sbuf(ph, "a_wq", [128, 8, 512], BF16)
            wgt_sb = sbuf(ph, "a_wgt", [128, 8, 24], BF16)
            wsr = sbuf(ph, "a_wsr", [128, 4, 128], F32)
            wtf = sbuf(ph, "a_wtf", [128, 4, 128], F32)
            WT = sbuf(ph, "a_WT", [128, 4, 128], BF16)
            LNG = sbuf(ph, "a_lng", [128, 512], F32)
            LNB = sbuf(ph, "a_lnb", [128, 512], F32)
            BSb = sbuf(ph, "a_bs", [128, 512], F32)
            x1t = [sbuf(ph, "a_x1t%d" % i, [128, 8, 128], F32) for i in range(2)]
            h2t_2 = [sbuf(ph, "a_h2t%d" % k_, [128, 8, 128], BF16) for k_ in range(2)]
            sq = sbuf(ph, "a_sq", [128, 8, 512], BF16)
            rs = sbuf(ph, "a_rs", [128, 512], F32)
            uT_2 = [sbuf(ph, "a_uT%d" % k_, [128, 512], F32) for k_ in range(2)]
            vg_2 = [sbuf(ph, "a_vg%d" % k_, [128, 512], F32) for k_ in range(2)]
            vcen_2 = [sbuf(ph, "a_vcen%d" % k_, [128, 512], F32) for k_ in range(2)]
            vsq_2 = [sbuf(ph, "a_vsq%d" % k_, [128, 512], F32) for k_ in range(2)]
            vln_2 = [sbuf(ph, "a_vln%d" % k_, [128, 512], BF16) for k_ in range(2)]
            ya_2 = [sbuf(ph, "a_ya%d" % k_, [128, 512], F32) for k_ in range(2)]
            yan_2 = [sbuf(ph, "a_yan%d" % k_, [128, 4, 128], BF16) for k_ in range(2)]
            st4_2 = [sbuf(ph, "a_st4%d" % k_, [128, 16], F32) for k_ in range(2)]
            B_w = Buf("a_w")
            B_wsr, B_WT = Buf("wsr"), Buf("WT")
            B_x1t = [Buf("x1t0"), Buf("x1t1")]
            B_rs = Buf("rs")
            B2 = {n: [Buf(n + "0"), Buf(n + "1")] for n in ("h2t", "uT", "vg", "vcen", "vsq", "vln", "ya", "yan", "st4")}
            sqb = [Buf("asq%d" % i) for i in range(8)]
            P.dma('pool', wu_sb[:], win_d[:, C_U:C_U + 512].rearrange("(kc p) m -> p kc m", p=128), writes=[B_w])
            P.dma('pool', wv_sb[:], win_d[:, C_V:C_V + 512].rearrange("(kc p) m -> p kc m", p=128), writes=[B_w])
            P.dma('pool', wq_sb[:], win_d[:, C_Q:C_Q + 512].rearrange("(kc p) m -> p kc m", p=128), writes=[B_w])
            P.dma('pool', wgt_sb[:], win_d[:, C_GT:C_GT + 24].rearrange("(kc p) m -> p kc m", p=128), writes=[B_w])
            P.dma('sp', wsr[:], ws_d[:, :, :].rearrange("g p q -> p g q"), writes=[B_wsr])
            B_w2 = Buf("a_w2")
            P.dma('sp', LNG[:], rowv_d[0:1, R_LNG:R_LNG + 512].partition_broadcast(128), writes=[B_w2])
            P.dma('sp', LNB[:], rowv_d[0:1, R_LNB:R_LNB + 512].partition_broadcast(128), writes=[B_w2])
            P.dma('sp', BSb[:], rowv_d[0:1, R_BS:R_BS + 512].partition_broadcast(128), writes=[B_w2])
            for g in range(4):
                P.op('pe', lambda e, g=g: e.transpose(ps[7][:, g * 128:(g + 1) * 128], wsr[:, g, :], ident[:]), reads=[B_wsr, B_const], writes=[psb[7]])
            P.op('dve', lambda e: e.tensor_copy(out=wtf[:].rearrange("p g q -> p (g q)"), in_=ps[7][:]), reads=[psb[7]], writes=[B_WT])
            P.op('pool', lambda e: e.affine_select(out=WT[:], in_=wtf[:], pattern=[[0, 4], [1, 128]], compare_op=ALU.is_ge, fill=0.0,
                                                   base=0, channel_multiplier=-1), reads=[B_WT], writes=[B_WT])

            def tile_front(i):
                xi = i % 2
                tsl = slice(i * 128, (i + 1) * 128)
                h2t, uT, vg, vcen, vsq, vln, ya, yan, st4 = [d_[xi] for d_ in (h2t_2, uT_2, vg_2, vcen_2, vsq_2, vln_2, ya_2, yan_2, st4_2)]
                B_h2t, B_uT, B_vg, B_vcen, B_vsq, B_vln, B_ya, B_yan, B_st4 = [B2[n][xi] for n in ("h2t", "uT", "vg", "vcen", "vsq", "vln", "ya", "yan", "st4")]
                P.dma('sp', x1t[xi][:], x1_d[:, :, tsl], reads=[x1db], writes=[B_x1t[xi]], parallel=False)
                rms_fm(x1t[xi], B_x1t[xi], G_MIXPRE, h2t, B_h2t, 128, sq, sqb, rs, B_rs)
                for r in range(4):
                    pi = r % 2
                    for k in range(8):
                        P.op('pe', lambda e, k=k, r=r, pi=pi: e.matmul(ps[pi][:, 0:128], lhsT=wq_sb[:, k, r * 128:(r + 1) * 128], rhs=h2t[:, k, :],
                                                                     start=(k == 0), stop=(k == 7)), reads=[B_w, B_h2t], writes=[psb[pi]])
                    P.op('act', lambda e, r=r, pi=pi, tsl=tsl: e.activation(out=QT[:, r, tsl], in_=ps[pi][:, 0:128], func=AF.Copy), reads=[psb[pi]], writes=[B_QT])
                for k in range(8):
                    P.op('pe', lambda e, k=k: e.matmul(ps[2][:, 0:24], lhsT=h2t[:, k, :], rhs=wgt_sb[:, k, :], start=(k == 0), stop=(k == 7)),
                         reads=[B_w, B_h2t], writes=[psb[2]])
                P.op('act', lambda e, i=i: e.activation(out=sig[:, i, :], in_=ps[2][:, 0:24], func=AF.Sigmoid), reads=[psb[2]], writes=[B_sig])
                for g in range(4):
                    for k in range(8):
                        P.op('pe', lambda e, k=k, g=g: e.matmul(ps[3][:, g * 128:(g + 1) * 128], lhsT=wu_sb[:, k, g * 128:(g + 1) * 128], rhs=h2t[:, k, :],
                                                              start=(k == 0), stop=(k == 7)), reads=[B_w, B_h2t], writes=[psb[3]])
                P.op('act', lambda e: e.activation(out=uT[:], in_=ps[3][:], func=AF.Gelu), reads=[psb[3]], writes=[B_uT])
                for k in range(8):
                    P.op('pe', lambda e, k=k: e.matmul(ps[4][:], lhsT=h2t[:, k, :], rhs=wv_sb[:, k, :], start=(k == 0), stop=(k == 7)),
                         reads=[B_w, B_h2t], writes=[psb[4]])
                P.op('act', lambda e: e.activation(out=vg[:], in_=ps[4][:], func=AF.Gelu), reads=[psb[4]], writes=[B_vg])

            def tile_back(i):
                xi = i % 2
                tsl = slice(i * 128, (i + 1) * 128)
                h2t, uT, vg, vcen, vsq, vln, ya, yan, st4 = [d_[xi] for d_ in (h2t_2, uT_2, vg_2, vcen_2, vsq_2, vln_2, ya_2, yan_2, st4_2)]
                B_h2t, B_uT, B_vg, B_vcen, B_vsq, B_vln, B_ya, B_yan, B_st4 = [B2[n][xi] for n in ("h2t", "uT", "vg", "vcen", "vsq", "vln", "ya", "yan", "st4")]
                vg3 = vg[:].rearrange("p (g c) -> p g c", g=4)
                vc3 = vcen[:].rearrange("p (g c) -> p g c", g=4)
                vs3 = vsq[:].rearrange("p (g c) -> p g c", g=4)
                P.op('dve', lambda e: e.tensor_reduce(out=st4[:, 0:4], in_=vg3, axis=AX.X, op=ALU.add), reads=[B_vg], writes=[B_st4])
                P.op('dve', lambda e: e.tensor_scalar(out=st4[:, 4:8], in0=st4[:, 0:4], scalar1=-1.0 / 128, scalar2=None, op0=ALU.mult), reads=[B_st4], writes=[B_st4])
                P.op('dve', lambda e: e.tensor_tensor(out=vc3, in0=vg3, in1=st4[:, 4:8].unsqueeze(2).to_broadcast([128, 4, 128]), op=ALU.add),
                     reads=[B_vg, B_st4], writes=[B_vcen])
                P.op('act', lambda e: e.activation(out=vsq[:], in_=vcen[:], func=AF.Square), reads=[B_vcen], writes=[B_vsq])
                P.op('dve', lambda e: e.tensor_reduce(out=st4[:, 8:12], in_=vs3, axis=AX.X, op=ALU.add), reads=[B_vsq, B_st4], writes=[B_st4])
                P.op('act', lambda e: e.activation(out=st4[:, 12:16], in_=st4[:, 8:12], func=AF.Ln, scale=1.0 / 128, bias=EPS), reads=[B_st4], writes=[B_st4])
                P.op('act', lambda e: e.activation(out=st4[:, 12:16], in_=st4[:, 12:16], func=AF.Exp, scale=-0.5), reads=[B_st4], writes=[B_st4])
                P.op('dve', lambda e: e.tensor_tensor(out=vs3, in0=vc3, in1=st4[:, 12:16].unsqueeze(2).to_broadcast([128, 4, 128]), op=ALU.mult),
                     reads=[B_vcen, B_st4, B_vsq], writes=[B_vsq])
                P.op('dve', lambda e: e.tensor_tensor(out=vsq[:], in0=vsq[:], in1=LNG[:], op=ALU.mult), reads=[B_vsq, B_w2], writes=[B_vsq])
                P.op('dve', lambda e: e.tensor_tensor(out=vln[:], in0=vsq[:], in1=LNB[:], op=ALU.add), reads=[B_vsq, B_w2], writes=[B_vln])
                for g in range(4):
                    P.op('pe', lambda e, g=g: e.matmul(ps[5][:, g * 128:(g + 1) * 128], lhsT=vln[:, g * 128:(g + 1) * 128], rhs=WT[:, g, :], start=True, stop=True),
                         reads=[B_vln, B_WT], writes=[psb[5]])
                P.op('dve', lambda e: e.tensor_tensor(out=ya[:], in0=ps[5][:], in1=BSb[:], op=ALU.add), reads=[psb[5], B_w2], writes=[B_ya])
                P.op('dve', lambda e: e.tensor_tensor(out=ya[:], in0=ya[:], in1=uT[:], op=ALU.mult), reads=[B_ya, B_uT], writes=[B_ya])
                for g in range(4):
                    P.op('act', lambda e, g=g: e.activation(out=sq[:, g, 0:128], in_=ya[:, g * 128:(g + 1) * 128], func=AF.Square), reads=[B_ya], writes=[sqb[g]])
                    P.op('pe', lambda e, g=g: e.matmul(ps[6][:, 0:128], lhsT=ones_bf[:], rhs=sq[:, g, 0:128], start=(g == 0), stop=(g == 3)),
                         reads=[sqb[g], B_const], writes=[psb[6]])
                rstd_from_stat(None, ps[6], psb[6], rs, B_rs, 512.0, 128)
                for g in range(4):
                    P.op('dve', lambda e, g=g: e.scalar_tensor_tensor(out=yan[:, g, :], in0=ya[:, g * 128:(g + 1) * 128], scalar=gcol[:, G_OGA + g:G_OGA + g + 1],
                                                                    in1=rs[:, 0:128], op0=ALU.mult, op1=ALU.mult),
                         reads=[B_ya, B_rs, B_gcol], writes=[B_yan])
                P.dma('sp', yn_d[:, 0:4, tsl], yan[:], reads=[B_yan], writes=[B_ynd], sem_owner=B_yan)
            tile_front(0)
            for i in range(NOWN):
                if i + 1 < NOWN:
                    tile_front(i + 1)
                tile_back(i)
            P.barrier()
            P.emit()

        if stage <= 3:
            if debug:
                dq = nc.dram_tensor("dbg_qt", [128, 4 * NOWN * 128], BF16, kind="ExternalOutput")
                dsg = nc.dram_tensor("dbg_sig", [128, NOWN * 24], F32, kind="ExternalOutput")
                dk = nc.dram_tensor("dbg_kct", [128, 512], BF16, kind="ExternalOutput")
                dv = nc.dram_tensor("dbg_vc", [128, 4 * 2 * 65], BF16, kind="ExternalOutput")
                dvn = nc.dram_tensor("dbg_vcn", [16, NOWN * 2 * 65], BF16, kind="ExternalOutput")
                P.dma('sp', dq[:, :], QT[:].rearrange("p r q -> p (r q)"), reads=[B_QT], writes=[Buf("dq")], is_output=True)
                P.dma('sp', dsg[:, :], sig[:].rearrange("p i c -> p (i c)"), reads=[B_sig], writes=[Buf("dsg")], is_output=True)
                P.dma('sp', dk[:, :], KcT[:], reads=[B_KcT], writes=[Buf("dk")], is_output=True)
                P.dma('sp', dv[:, :], Vc[:].rearrange("p a b c -> p (a b c)"), reads=[B_Vc], writes=[Buf("dv")], is_output=True)
                P.dma('sp', dvn[:, :], VcN[:].rearrange("p a b c -> p (a b c)"), reads=[B_VcN], writes=[Buf("dvn")], is_output=True)
                P.barrier()
                P.emit()
            att.close()
            return dbg_exit()

        with ExitStack() as ph:
            tab_sb = sbuf(ph, "b_tab", [32, 8], F32)
            negrow = sbuf(ph, "b_neg", [1, 8], F32)
            ohd_sb = sbuf(ph, "b_ohd", [32, 768], F32)
            ohm_sb = sbuf(ph, "b_ohm", [1, 768], F32)
            fv_sb = sbuf(ph, "b_fv", [8, 768], F32)
            Hs = sbuf(ph, "b_Hs", [128, 8, 3, 128], F32)
            H2s = sbuf(ph, "b_H2s", [16, 8, 128], F32)
            BIAS = sbuf(ph, "b_BIAS", [128, 2, 3, 512], F32)
            BN = sbuf(ph, "b_BN", [16, 2, 512], F32)
            RQ = [[sbuf(ph, "b_RQ%d%d" % (a_, b_), [128, 4, 128], BF16) for b_ in range(2)] for a_ in range(2)]
            B_RQ = [[Buf("RQ%d%d" % (a_, b_)) for b_ in range(2)] for a_ in range(2)]
            selsw = sbuf(ph, "b_selsw", [128, 128], F32)
            B_selsw = Buf("selsw")
            SMASK = sbuf(ph, "b_smask", [128, NOWN, 2, 128], F32)
            KB = sbuf(ph, "b_kb", [128, NT], F32)
            KBC = sbuf(ph, "b_kbc", [128, 4], F32)
            KBN = sbuf(ph, "b_kbn", [16, NOWN], F32)
            OVF = sbuf(ph, "b_ovf", [128, 4, 128], BF16)
            OVN = sbuf(ph, "b_ovn", [16, NOWN, 128], BF16)
            OGB = sbuf(ph, "b_ogb", [128, 512], F32)
            Pt = [sbuf(ph, "b_Pt%d" % i, [128, 512], BF16) for i in range(4)]
            tmpS = [sbuf(ph, "b_tmpS%d" % i, [128, 512], F32) for i in range(2)]
            rsbt = sbuf(ph, "b_rsb", [128, 512], F32)
            imp4 = sbuf(ph, "b_imp4", [128, 512], F32)
            impT = sbuf(ph, "b_impT", [128, 128], F32)
            score = sbuf(ph, "b_score", [128, 128], F32)
            sc2 = sbuf(ph, "b_sc2", [128, 128], F32)
            selt = sbuf(ph, "b_sel", [128, 128], F32)
            m8 = sbuf(ph, "b_m8", [128, 24], F32)
            negsel = sbuf(ph, "b_negsel", [128, 4, 128], BF16)
            cf = sbuf(ph, "b_cf", [128, 16], F32)
            ybt = sbuf(ph, "b_yb", [128, 8, 64], F32)
            ybn = sbuf(ph, "b_ybn", [128, 512], F32)
            tmpo = sbuf(ph, "b_tmpo", [128, 4, 64], F32)
            ybT = sbuf(ph, "b_ybT", [128, 4, 128], BF16)
            B_c2 = Buf("b_const")
            B_fv, B_fvd, B_Hs, B_BIAS = Buf("fv"), Buf("fvd"), Buf("Hs"), Buf("BIAS")
            B_Pt = [Buf("Pt%d" % i) for i in range(4)]
            B_tmpS = [Buf("tmpS0"), Buf("tmpS1")]
            B_rsb, B_imp4, B_impT, B_score, B_sc2, B_sel, B_m8, B_negsel, B_cf, B_yb, B_ybn, B_tmpo, B_ybT = [Buf(n) for n in (
                "rsb", "imp4", "impT", "score", "sc2", "sel", "m8", "negsel", "cf", "yb", "ybn", "tmpo", "ybT")]
            P.dma('sp', tab_sb[:], tab_d[:, :], writes=[B_c2])
            P.dma('sp', ohd_sb[:], ohd_d[0:32, :], writes=[B_c2])
            P.dma('sp', ohm_sb[:], ohd_d[32:33, :], writes=[B_c2])
            P.dma('sp', SMASK[:], smask_d[:, :, :, :], writes=[B_c2])
            P.dma('sp', KB[:], kb_d[:, :], writes=[B_c2])
            P.dma('sp', KBC[:], kbc_d[:, :], writes=[B_c2])
            P.dma('sp', KBN[:], kbn_d[:, :], writes=[B_c2])
            B_c2p = Buf("b_constp")
            P.dma('pool', OVF[:], ovf_d[:, :, :], writes=[B_c2p])
            P.dma('pool', OVN[:], ovn_d[:, :, :], writes=[B_c2p])
            P.dma('sp', OGB[:], rowv_d[0:1, R_OGB:R_OGB + 512].partition_broadcast(128), writes=[B_c2])
            P.op('pool', lambda e: e.memset(negrow[:], NEG), writes=[B_c2])
            for half in range(2):
                cs = slice(half * 384, (half + 1) * 384)
                P.op('pe', lambda e, cs=cs: e.matmul(ps[0][0:8, 0:384], lhsT=tab_sb[:, :], rhs=ohd_sb[:, cs], start=True, stop=False), reads=[B_c2], writes=[psb[0]])
                P.op('pe', lambda e, cs=cs: e.matmul(ps[0][0:8, 0:384], lhsT=negrow[:, :], rhs=ohm_sb[:, cs], start=False, stop=True), reads=[B_c2], writes=[psb[0]])
                P.op('dve', lambda e, cs=cs: e.tensor_copy(out=fv_sb[:, cs], in_=ps[0][0:8, 0:384]), reads=[psb[0]], writes=[B_fv])
            P.dma('sp', fv_d[:, :], fv_sb[:], reads=[B_fv], writes=[B_fvd])
            for h in range(8):
                for di, dl in enumerate((0, 1, 4)):
                    src = bass.AP(tensor=fv_d, offset=h * 768 + dl * 128, ap=[[1, 128], [1, 128]])
                    P.dma('sp', Hs[:, h, di, :], src, reads=[B_fvd], writes=[B_Hs])
                src = bass.AP(tensor=fv_d, offset=h * 768, ap=[[16, 16], [1, 128]])
                P.dma('sp', H2s[:, h, :], src, reads=[B_fvd], writes=[B_Hs])
            for g in range(2):
                for di in range(3):
                    pi = (g * 3 + di) % 2
                    P.op('pe', lambda e, g=g, di=di, pi=pi: e.matmul(ps[pi][:], lhsT=antid[:, :], rhs=Hs[:, 4 * g:4 * g + 4, di, :], start=True, stop=True),
                         reads=[B_Hs, B_const], writes=[psb[pi]])
                    P.op('act', lambda e, g=g, di=di, pi=pi: e.activation(out=BIAS[:, g, di, :], in_=ps[pi][:], func=AF.Copy), reads=[psb[pi]], writes=[B_BIAS])
                P.op('pe', lambda e, g=g: e.matmul(ps[2][0:16, :], lhsT=antid[0:16, 112:128], rhs=H2s[0:16, 4 * g:4 * g + 4, :], start=True, stop=True),
                     reads=[B_Hs, B_const], writes=[psb[2]])
                P.op('act', lambda e, g=g: e.activation(out=BN[0:16, g, :], in_=ps[2][0:16, :], func=AF.Copy), reads=[psb[2]], writes=[B_BIAS])

            state = {'pt': 0, 'tmp': 0, 's': 0}
            NPT = 4

            def unit(i, g, kT_ap, w, v_ap, kvbufs, o_i, first, last, bias_ap=None, kbias_ap=None, mask_kt=None, ov_ap=None, scale=SCALE, sbanks=(0, 1)):
                si = sbanks[state['s'] % len(sbanks)]
                state['s'] += 1
                pti = state['pt'] % NPT
                state['pt'] += 1
                if mask_kt is None:
                    qrhs = QT[64 * g:64 * g + 64, :, i * 128:(i + 1) * 128]
                    qb_ = [B_QT]
                else:
                    rq_a, rq_b = mask_kt
                    qrhs = RQ[rq_a][rq_b][:]
                    qb_ = [B_RQ[rq_a][rq_b]]
                P.op('pe', lambda e: e.matmul(ps[si][:w, :], lhsT=kT_ap, rhs=qrhs, start=True, stop=True), reads=kvbufs + qb_, writes=[psb[si]])
                kb_ = kbias_ap if kbias_ap is not None else 0.0
                if bias_ap is not None:
                    ti = state['tmp'] % 2
                    state['tmp'] += 1
                    P.op('dve', lambda e: e.scalar_tensor_tensor(out=tmpS[ti][:w, :], in0=ps[si][:w, :], scalar=scale, in1=bias_ap, op0=ALU.mult, op1=ALU.add),
                         reads=[psb[si], B_BIAS], writes=[B_tmpS[ti]])
                    P.op('act', lambda e: e.activation(out=Pt[pti][:w, :], in_=tmpS[ti][:w, :], func=AF.Exp, bias=kb_), reads=[B_tmpS[ti], B_c2, B_const], writes=[B_Pt[pti]])
                else:
                    P.op('act', lambda e: e.activation(out=Pt[pti][:w, :], in_=ps[si][:w, :], func=AF.Exp, scale=scale, bias=kb_),
                         reads=[psb[si], B_c2, B_const], writes=[B_Pt[pti]])
                def pv():
                    for r in range(4):
                        P.op('pe', lambda e, r=r: e.matmul(ps[o_i][:, r * 65:(r + 1) * 65], lhsT=Pt[pti][:w, r * 128:(r + 1) * 128], rhs=v_ap, start=(first and r == 0), stop=last, skip_group_check=True),
                             reads=[B_Pt[pti]] + kvbufs, writes=[psb[o_i]])
                    if ov_ap is not None:
                        P.op('pe', lambda e: e.matmul(ps[5][:], lhsT=ov_ap, rhs=Pt[pti][:w, :], start=first, stop=last), reads=[B_Pt[pti], B_c2p], writes=[psb[5]])
                        P.op('pe', lambda e: e.matmul(ps[6][:], lhsT=ones_bf[:w, :], rhs=Pt[pti][:w, :], start=first, stop=last), reads=[B_Pt[pti], B_const], writes=[psb[6]])
                return pv

            def run_units(specs, skew=1, hooks=None):
                pend = []
                for n_, (args, kw) in enumerate(specs):
                    pend.append(unit(*args, **kw))
                    if len(pend) > skew:
                        pend.pop(0)()
                    if hooks and n_ in hooks:
                        hooks[n_]()
                while pend:
                    pend.pop(0)()

            def combine(i, g, o_i, br, first_branch):
                ybt, B_yb = ybt2[i % 2], B_yb2[i % 2]
                o3 = ps[o_i][:, 0:260].rearrange("p (r e) -> p r e", e=65)
                gate = sig[:, i, :].rearrange("p (h b) -> p h b", b=3)[:, 4 * g:4 * g + 4, br]
                cs = slice(br * 4, br * 4 + 4)
                P.op('dve', lambda e: e.tensor_scalar(out=cf[:, cs], in0=o3[:, :, 64], scalar1=1e-30, scalar2=None, op0=ALU.max), reads=[psb[o_i]], writes=[B_cf])
                P.op('dve', lambda e: e.reciprocal(out=cf[:, cs], in_=cf[:, cs]), reads=[B_cf], writes=[B_cf])
                P.op('dve', lambda e: e.tensor_tensor(out=cf[:, cs], in0=cf[:, cs], in1=gate, op=ALU.mult), reads=[B_cf, B_sig], writes=[B_cf])
                cb = cf[:, cs].unsqueeze(2).to_broadcast([128, 4, 64])
                if first_branch:
                    P.op('dve', lambda e: e.tensor_tensor(out=ybt[:, 4 * g:4 * g + 4, :], in0=o3[:, :, 0:64], in1=cb, op=ALU.mult), reads=[psb[o_i], B_cf], writes=[B_yb])
                else:
                    P.op('dve', lambda e: e.tensor_tensor(out=tmpo[:], in0=o3[:, :, 0:64], in1=cb, op=ALU.mult), reads=[psb[o_i], B_cf], writes=[B_tmpo])
                    P.op('dve', lambda e: e.tensor_tensor(out=ybt[:, 4 * g:4 * g + 4, :], in0=ybt[:, 4 * g:4 * g + 4, :], in1=tmpo[:], op=ALU.add),
                         reads=[B_tmpo, B_yb], writes=[B_yb])

            n_own = NOWN if stage >= 5 else 2
            ybt2 = [ybt, sbuf(ph, "b_yb1", [128, 8, 64], F32)]
            B_yb2 = [B_yb, Buf("yb1")]

            def cw_units(i, g):
                qt = 4 * i + 3
                gs = slice(64 * g, 64 * g + 64)
                for b_ in range(2):
                    P.op('pool', lambda e, b_=b_: e.tensor_copy(out=RQ[(2 * i + g) % 2][b_][gs, :, :], in_=QT[gs, :, i * 128:(i + 1) * 128]),
                         reads=[B_QT, B_RQ[(2 * i + g) % 2][b_]], writes=[B_RQ[(2 * i + g) % 2][b_]])
                nfar = 8 * qt - 9
                c0 = nfar
                tiles = [(ct, 128) for ct in range(nfar // 128)]
                if nfar % 128:
                    tiles.append((nfar // 128, nfar % 128))
                specs = []
                for n_, (ct, w) in enumerate(tiles):
                    specs.append(((i, g, KcT[gs, ct * 128:ct * 128 + w], w, Vc[:w, ct, g, :], [B_KcT, B_Vc], 2, n_ == 0, False),
                                  dict(kbias_ap=KBC[:w, ct:ct + 1], ov_ap=OVF[:w, ct, :])))
                specs.append(((i, g, KcT[gs, c0:c0 + 16], 16, VcN[0:16, i, g, :], [B_KcT, B_VcN], 2, False, True),
                              dict(bias_ap=BN[0:16, g, :], kbias_ap=KBN[0:16, i:i + 1], ov_ap=OVN[0:16, i, :])))
                wl = [dl for dl in (4, 3, 2, 1, 0) if qt - dl >= 0]
                for n_, dl in enumerate(wl):
                    kt = qt - dl
                    bias_ap = BIAS[:, g, {0: 0, 1: 1, 4: 2}[dl], :] if dl in (0, 1, 4) else None
                    specs.append(((i, g, kwnT[gs, kt * 128:(kt + 1) * 128], 128, vwn[:, kt, g, :], [B_kwn, B_vwn], 4, n_ == 0, n_ == len(wl) - 1),
                                  dict(bias_ap=bias_ap, kbias_ap=KB[:, kt:kt + 1])))
                run_units(specs)

            def chain_a(i, g):
                P.op('dve', lambda e: e.tensor_scalar(out=rsbt[:], in0=ps[6][:], scalar1=1e-18, scalar2=None, op0=ALU.max), reads=[psb[6]], writes=[B_rsb])
                P.op('act', lambda e: e.activation(out=rsbt[:], in_=rsbt[:], func=AF.Ln), reads=[B_rsb], writes=[B_rsb])
                P.op('act', lambda e: e.activation(out=rsbt[:], in_=rsbt[:], func=AF.Exp, scale=-1.0), reads=[B_rsb], writes=[B_rsb])
                P.op('dve', lambda e: e.tensor_tensor(out=imp4[:], in0=ps[5][:], in1=rsbt[:], op=ALU.mult), reads=[psb[5], B_rsb], writes=[B_imp4])
                P.op('dve', lambda e: e.tensor_reduce(out=impT[:], in_=imp4[:].rearrange("p (r q) -> p q r", r=4), axis=AX.X, op=ALU.add),
                     reads=[B_imp4], writes=[B_impT])

            def chain_t1(i, g):
                P.op('pe', lambda e: e.transpose(ps[7][:, 0:128], impT[:], ident[:]), reads=[B_impT, B_const], writes=[psb[7]])
                P.op('dve', lambda e: e.tensor_tensor(out=score[:], in0=ps[7][:, 0:128], in1=SMASK[:, i, 0, :], op=ALU.mult), reads=[psb[7], B_c2], writes=[B_score])
                P.op('dve', lambda e: e.tensor_tensor(out=score[:], in0=score[:], in1=SMASK[:, i, 1, :], op=ALU.add), reads=[B_score, B_c2], writes=[B_score])
                P.op('dve', lambda e: e.max(out=m8[:, 0:8], in_=score[:]), reads=[B_score], writes=[B_m8])
                P.op('dve', lambda e: e.match_replace(out=sc2[:], in_to_replace=m8[:, 0:8], in_values=score[:], imm_value=-1e30), reads=[B_score, B_m8], writes=[B_sc2])
                P.op('dve', lambda e: e.max(out=m8[:, 8:16], in_=sc2[:]), reads=[B_sc2, B_m8], writes=[B_m8])
                P.op('dve', lambda e: e.tensor_scalar(out=m8[:, 16:17], in0=m8[:, 15:16], scalar1=0.0, scalar2=None, op0=ALU.max), reads=[B_m8], writes=[B_m8])
                P.op('dve', lambda e: e.tensor_scalar(out=selt[:], in0=score[:], scalar1=m8[:, 16:17], scalar2=None, op0=ALU.is_ge), reads=[B_score, B_m8], writes=[B_sel])
                P.op('dve', lambda e: e.tensor_copy(out=selsw[:, 0:64], in_=selt[:, 64:128]), reads=[B_sel], writes=[B_selsw])
                P.op('dve', lambda e: e.tensor_copy(out=selsw[:, 64:128], in_=selt[:, 0:64]), reads=[B_sel, B_selsw], writes=[B_selsw])

            def chain_t23(i, g):
                rqa = (2 * i + g) % 2
                oth = slice(64, 128) if g == 0 else slice(0, 64)
                P.op('pe', lambda e: e.transpose(ps[7][:, 128:256], selt[:], ident[:]), reads=[B_sel, B_const], writes=[psb[7]])
                P.op('pe', lambda e: e.transpose(ps[7][:, 256:384], selsw[:], ident[:]), reads=[B_selsw, B_const], writes=[psb[7]])
                src_lo = ps[7][oth, 256:384] if g == 0 else ps[7][oth, 128:256]
                src_hi = ps[7][oth, 128:256] if g == 0 else ps[7][oth, 256:384]
                for b_, src_ in ((0, src_lo), (1, src_hi)):
                    P.op('dve', lambda e, b_=b_, src_=src_: e.tensor_scalar(
                        out=RQ[rqa][b_][oth, :, :], in0=src_.unsqueeze(1).to_broadcast([64, 4, 128]), scalar1=-1.0, scalar2=BIGNEG, op0=ALU.add, op1=ALU.mult),
                        reads=[psb[7], B_RQ[rqa][b_]], writes=[B_RQ[rqa][b_]])

            def sel_units(i, g, hooks):
                qt = 4 * i + 3
                specs = []
                for kt in range(qt + 1):
                    dl = qt - kt
                    bias_ap = BIAS[:, g, dl, :] if dl <= 1 else None
                    specs.append(((i, g, KE[g][:, kt * 128:(kt + 1) * 128], 128, vsl[:, kt, g, :], [B_ksl, B_vsl], 3, kt == 0, kt == qt),
                                  dict(bias_ap=bias_ap, mask_kt=((2 * i + g) % 2, 0 if kt < 32 else 1), sbanks=(0, 1))))
                run_units(specs, skew=1, hooks=hooks)

            def finish_sel(i, g):
                combine(i, g, 3, 1, False)
                if g == 0:
                    return
                yb_t, yb_b = ybt2[i % 2], B_yb2[i % 2]
                tsl = slice(i * 128, (i + 1) * 128)
                P.op('act', lambda e: e.activation(out=ybn[:], in_=yb_t[:].rearrange("p h d -> p (h d)"), func=AF.Square, accum_out=cf[:, 12:13]), reads=[yb_b], writes=[B_ybn, B_cf])
                P.op('act', lambda e: e.activation(out=cf[:, 13:14], in_=cf[:, 12:13], func=AF.Ln, scale=1.0 / 512, bias=EPS), reads=[B_cf], writes=[B_cf])
                P.op('act', lambda e: e.activation(out=cf[:, 13:14], in_=cf[:, 13:14], func=AF.Exp, scale=-0.5), reads=[B_cf], writes=[B_cf])
                P.op('dve', lambda e: e.scalar_tensor_tensor(out=ybn[:], in0=yb_t[:].rearrange("p h d -> p (h d)"), scalar=cf[:, 13:14], in1=OGB[:], op0=ALU.mult, op1=ALU.mult),
                     reads=[yb_b, B_cf, B_c2, B_ybn], writes=[B_ybn])
                for cc in range(4):
                    P.op('pe', lambda e, cc=cc: e.transpose(ps[7][:, cc * 128:(cc + 1) * 128], ybn[:, cc * 128:(cc + 1) * 128], ident[:]), reads=[B_ybn, B_const], writes=[psb[7]])
                P.op('act', lambda e: e.activation(out=ybT[:].rearrange("p c q -> p (c q)"), in_=ps[7][:], func=AF.Copy), reads=[psb[7]], writes=[B_ybT])
                P.dma('sp', yn_d[:, 4:8, tsl], ybT[:], reads=[B_ybT], writes=[B_ynd], sem_owner=B_ybT)

            seq = [(i, g) for i in range(n_own) for g in range(2)]
            prev = None
            for (i, g) in seq:
                cw_units(i, g)
                chain_a(i, g)
                if prev is not None:
                    n_units = 4 * prev[0] + 4
                    sel_units(prev[0], prev[1], {n_units // 3: (lambda i=i, g=g: chain_t1(i, g)), (2 * n_units) // 3: (lambda i=i, g=g: chain_t23(i, g))})
                    finish_sel(*prev)
                else:
                    chain_t1(i, g)
                    chain_t23(i, g)
                combine(i, g, 2, 0, True)
                combine(i, g, 4, 2, False)
                prev = (i, g)
            sel_units(prev[0], prev[1], {})
            finish_sel(*prev)
            P.barrier()
            P.emit()
        att.close()

        if stage <= 5:
            return dbg_exit()

        XK = sbuf(es, "p3_XK", [128, 8, 256], BF16)
        XV = sbuf(es, "p3_XV", [128, 2, D], BF16)
        B_XK, B_XV = Buf("XK"), Buf("XV")
        with ExitStack() as pm:
            memt = sbuf(pm, "m_mem", [128, 2, D], F32)
            memn = sbuf(pm, "m_memn", [128, D], F32)
            MN = sbuf(pm, "m_MN", [128, D], F32)
            memnT = sbuf(pm, "m_memnT", [128, 8, 256], BF16)
            wv2 = sbuf(pm, "m_wv2", [128, 8, D], BF16)
            mst = sbuf(pm, "m_st", [128, 4], F32)
            B_mem, B_memn, B_memnT, B_wv2, B_mst = Buf("mem"), Buf("memn"), Buf("memnT"), Buf("wv2"), Buf("mst")
            P.dma('sp', memt[:], mem_d[:, :].rearrange("(t p) f -> p t f", p=128), writes=[B_mem])
            P.dma('sp', MN[:], rowv_d[0:1, R_MEMN:R_MEMN + D].partition_broadcast(128), writes=[B_mem])
            P.dma('pool', wv2[:], xwkv_d[:, D:2 * D].rearrange("(kc p) m -> p kc m", p=128), writes=[B_wv2])
            for mt in range(2):
                P.op('act', lambda e, mt=mt: e.activation(out=memn[:], in_=memt[:, mt, :], func=AF.Square, accum_out=mst[:, 0:1]), reads=[B_mem], writes=[B_memn, B_mst])
                P.op('act', lambda e: e.activation(out=mst[:, 1:2], in_=mst[:, 0:1], func=AF.Sqrt, scale=1.0 / D, bias=EPS), reads=[B_mst], writes=[B_mst])
                P.op('dve', lambda e: e.reciprocal(out=mst[:, 1:2], in_=mst[:, 1:2]), reads=[B_mst], writes=[B_mst])
                P.op('dve', lambda e, mt=mt: e.scalar_tensor_tensor(out=memn[:], in0=memt[:, mt, :], scalar=mst[:, 1:2], in1=MN[:], op0=ALU.mult, op1=ALU.mult),
                     reads=[B_mem, B_mst, B_memn], writes=[B_memn])
                for half in range(2):
                    for cc in range(4):
                        c = half * 4 + cc
                        P.op('pe', lambda e, c=c, cc=cc: e.transpose(ps[7][:, cc * 128:(cc + 1) * 128], memn[:, c * 128:(c + 1) * 128], ident[:]),
                             reads=[B_memn, B_const], writes=[psb[7]])
                    P.op('dve', lambda e, half=half, mt=mt: e.tensor_copy(out=memnT[:, half * 4:half * 4 + 4, mt * 128:(mt + 1) * 128],
                                                                        in_=ps[7][:].rearrange("p (c q) -> p c q", c=4)), reads=[psb[7]], writes=[B_memnT])
            m8s = WStream(pm, "m_w8", 8, 3)
            nxt = m8s.load(xwkv_d, 0)
            for e_ in range(8):
                wt, wb = nxt
                if e_ + 1 < 8:
                    nxt = m8s.load(xwkv_d, e_ + 1)
                pi = e_ % 2
                for k in range(8):
                    P.op('pe', lambda e, k=k, wt=wt, pi=pi: e.matmul(ps[pi][:, 0:256], lhsT=wt[:, k, :], rhs=memnT[:, k, :], start=(k == 0), stop=(k == 7)),
                         reads=[wb, B_memnT], writes=[psb[pi]])
                P.op('act', lambda e, e_=e_, pi=pi: e.activation(out=XK[:, e_, :], in_=ps[pi][:, 0:256], func=AF.Copy), reads=[psb[pi]], writes=[B_XK])
            for mt in range(2):
                for half in range(2):
                    pi = 2 + (mt * 2 + half) % 2
                    for k in range(8):
                        P.op('pe', lambda e, k=k, mt=mt, half=half, pi=pi: e.matmul(ps[pi][:], lhsT=memnT[:, k, mt * 128:(mt + 1) * 128],
                                                                                  rhs=wv2[:, k, half * 512:(half + 1) * 512], start=(k == 0), stop=(k == 7)),
                             reads=[B_wv2, B_memnT], writes=[psb[pi]])
                    P.op('act', lambda e, mt=mt, half=half, pi=pi: e.activation(out=XV[:, mt, half * 512:(half + 1) * 512], in_=ps[pi][:], func=AF.Copy),
                         reads=[psb[pi]], writes=[B_XV])
            P.barrier()
            P.emit()

        with ExitStack() as ph:
            xT = sbuf(ph, "p3_xT", [128, 8, SBN], F32)
            hT = sbuf(ph, "p3_hT", [128, 8, SBN], BF16)
            actT = sbuf(ph, "p3_actT", [128, NJ, SBN], BF16)
            yT = sbuf(ph, "p3_yT", [128, 8, SBN], F32)
            sq = sbuf(ph, "p3_sq", [128, 8, 512], BF16)
            rs = sbuf(ph, "p3_rs", [128, 512], F32)
            sg = [sbuf(ph, "p3_sg%d" % i, [128, 512], F32) for i in range(2)]
            xo = [sbuf(ph, "p3_xo%d" % i, [128, D], F32) for i in range(2)]
            Pm = [sbuf(ph, "p3_Pm%d" % i, [128, 512], BF16) for i in range(2)]
            xb, hb, actb, yb_, rsb = Buf("xT3"), Buf("hT3"), Buf("actT3"), Buf("yT3"), Buf("rs3")
            sqb = [Buf("sq3%d" % i) for i in range(8)]
            sgb = [Buf("sg30"), Buf("sg31")]
            xob = [Buf("xo0"), Buf("xo1")]
            B_Pm = [Buf("Pm0"), Buf("Pm1")]
            wgs = WStream(ph, "p3_wg", 8, 3)
            wus = WStream(ph, "p3_wu", 8, 3)
            wds = WStream(ph, "p3_wd", NJ, 2)
            w8s = WStream(ph, "p3_w8", 8, 3)
            for sbi in range(2):
                tok = slice(sbi * SBN, (sbi + 1) * SBN)
                P.dma('sp', xT[:], x1_d[:, :, tok], reads=[x1db], writes=[xb], parallel=False)
                P.dma('sp', hT[:], yn_d[:, :, tok], reads=[B_ynd], writes=[hb], parallel=False)
                lin_post(ph, hT, hb, 8, wout_d, w8s, xT, xb, SBN, lambda c: gcol[:, G_MIXPOST + c:G_MIXPOST + c + 1], yT, yb_, sq, sqb, rs, rsb)
                rms_fm(xT, xb, G_XAPRE, hT, hb, SBN, sq, sqb, rs, rsb)
                nxt = w8s.load(xwq_d, 0)
                for e_ in range(8):
                    wt, wb = nxt
                    if e_ + 1 < 8:
                        nxt = w8s.load(xwq_d, e_ + 1)
                    for tb in range(2):
                        t0 = tb * 512
                        pi = (e_ * 2 + tb) % 2
                        for k in range(8):
                            P.op('pe', lambda e, k=k, wt=wt, pi=pi, t0=t0: e.matmul(ps[pi][:], lhsT=wt[:, k, :], rhs=hT[:, k, t0:t0 + 512], start=(k == 0), stop=(k == 7)),
                                 reads=[wb, hb], writes=[psb[pi]])
                        P.op('act', lambda e, e_=e_, pi=pi, t0=t0: e.activation(out=actT[:, e_, t0:t0 + 512], in_=ps[pi][:], func=AF.Copy), reads=[psb[pi]], writes=[actb])
                for hh in range(4):
                    for tb in range(2):
                        t0 = tb * 512
                        for mt in range(2):
                            for dc in range(2):
                                P.op('pe', lambda e, hh=hh, mt=mt, dc=dc, t0=t0: e.matmul(ps[mt][:], lhsT=XK[:, 2 * hh + dc, mt * 128:(mt + 1) * 128],
                                                                                        rhs=actT[:, 2 * hh + dc, t0:t0 + 512], start=(dc == 0), stop=(dc == 1)),
                                     reads=[B_XK, actb], writes=[psb[mt]])
                            P.op('act', lambda e, mt=mt: e.activation(out=Pm[mt][:], in_=ps[mt][:], func=AF.Exp, scale=1.0 / 16), reads=[psb[mt]], writes=[B_Pm[mt]])
                        for mt in range(2):
                            P.op('pe', lambda e, mt=mt: e.matmul(ps[6][:], lhsT=ones_bf[:], rhs=Pm[mt][:], start=(mt == 0), stop=(mt == 1)),
                                 reads=[B_Pm[mt], B_const], writes=[psb[6]])
                        for dc in range(2):
                            for mt in range(2):
                                P.op('pe', lambda e, hh=hh, mt=mt, dc=dc: e.matmul(ps[2 + dc][:], lhsT=XV[:, mt, (2 * hh + dc) * 128:(2 * hh + dc + 1) * 128], rhs=Pm[mt][:],
                                                                                 start=(mt == 0), stop=(mt == 1)),
                                     reads=[B_XV, B_Pm[mt]], writes=[psb[2 + dc]])
                        P.op('act', lambda e: e.activation(out=rs[:], in_=ps[6][:], func=AF.Ln), reads=[psb[6]], writes=[rsb])
                        P.op('act', lambda e: e.activation(out=rs[:], in_=rs[:], func=AF.Exp, scale=-1.0), reads=[rsb], writes=[rsb])
                        for dc in range(2):
                            P.op('dve', lambda e, hh=hh, dc=dc, t0=t0: e.tensor_tensor(out=actT[:, 8 + 2 * hh + dc, t0:t0 + 512], in0=ps[2 + dc][:], in1=rs[:], op=ALU.mult),
                                 reads=[psb[2 + dc], rsb, actb], writes=[actb])
                lin_post(ph, actT[:, 8:16, :], actb, 8, xwo_d, w8s, xT, xb, SBN, lambda c: gcol[:, G_XAPOST + c:G_XAPOST + c + 1], yT, yb_, sq, sqb, rs, rsb)
                ffn(ph, 1, xT, xb, hT, hb, actT, actb, yT, yb_, sq, sqb, rs, rsb, sg, sgb, wgs, wus, wds, G_F2PRE, 8)
                for t in range(SBT):
                    oi = t % 2
                    for half in range(2):
                        for cc in range(4):
                            c = half * 4 + cc
                            P.op('pe', lambda e, c=c, cc=cc, t=t: e.transpose(ps[7][:, cc * 128:(cc + 1) * 128], xT[:, c, t * 128:(t + 1) * 128], ident[:]),
                                 reads=[xb, B_const], writes=[psb[7]])
                        P.op('act', lambda e, half=half, oi=oi: e.activation(out=xo[oi][:, half * 512:(half + 1) * 512], in_=ps[7][:], func=AF.Copy),
                             reads=[psb[7]], writes=[xob[oi]])
                    row = (sbi * SBT + t) * 128
                    P.dma('sp', y_d[row:row + 128, :], xo[oi][:], reads=[xob[oi]], writes=[Buf("yout%d" % (sbi * SBT + t))], is_output=True)
            P.finish()
            P.emit()
    return nc


def _t5_bucket(d):
    d = np.maximum(d, 0)
    nf = np.maximum(d, 1).astype(np.float32)
    large = 16 + (np.log(nf / np.float32(16)) / np.float32(math.log(8.0)) * np.float32(16)).astype(np.int32)
    large = np.minimum(large, 31)
    return np.where(d < 16, d, large)


def _consts(r):
    sh_t = 3 - r
    shift = 128 * sh_t
    j0 = 2 * sh_t
    c_first = 8 * sh_t
    ohd = np.zeros((33, 768), np.float32)
    n = np.arange(768)
    d = n - 127
    valid = (d >= 0) & (d < 512)
    bk = _t5_bucket(d)
    ohd[bk[valid], n[valid]] += 1.0
    ohd[31, n[valid]] -= 1.0
    ohd[32, ~valid] = 1.0
    smask = np.zeros((128, NOWN, 2, 128), np.float32)
    jj = np.arange(128)[None, :]
    for i in range(NOWN):
        t = 128 * (4 * i + 3) + np.arange(128)[:, None]
        cur = t // 64
        exists = jj >= j0
        causal = (jj <= cur) & exists
        forced = ((jj == j0) | (jj == cur) | (jj == cur - 1)) & exists
        smask[:, i, 0, :] = (causal & ~forced).astype(np.float32)
        smask[:, i, 1, :] = np.where(forced, 1e4, np.where(causal, 0.0, -1.0))
    kb = np.zeros((128, NT), np.float32)
    pos = 128 * np.arange(NT)[None, :] + np.arange(128)[:, None]
    kb[pos < shift] = NEG
    kbc = np.zeros((128, 4), np.float32)
    cc = 128 * np.arange(4)[None, :] + np.arange(128)[:, None]
    kbc[cc < c_first] = NEG
    kbn = np.zeros((16, NOWN), np.float32)
    cn = (8 * (4 * np.arange(NOWN) + 3) - 9)[None, :] + np.arange(16)[:, None]
    kbn[cn < c_first] = NEG
    def ov(c, j):
        return np.clip(np.minimum(c + 2, 4 * (j + 1)) - np.maximum(c, 4 * j), 0, None).astype(np.float32)
    ovf = ov(cc[:, :, None], np.arange(128)[None, None, :])
    ovn = ov(cn[:, :, None], np.arange(128)[None, None, :])
    return dict(ohd=ohd, smask=smask, kb=kb, kbc=kbc, kbn=kbn, ovf=ovf.astype(np.float32), ovn=ovn.astype(np.float32))


def _col(v):
    return np.ascontiguousarray(np.asarray(v, np.float32).reshape(-1, 128).T)


STAGE = 99
DEBUG = False
_last = {}


def kernel(**inp):
    f = lambda k: np.asarray(inp[k], np.float32)
    x = f('x')
    mem = f('mem')
    w_in = f('w_in')[0]
    qperm = np.arange(512).reshape(2, 4, 64).transpose(1, 0, 2).reshape(-1)
    w_in_r = np.ascontiguousarray(np.concatenate([
        w_in[:, 0:1024], w_in[:, 1024:1536][:, qperm], w_in[:, 1536:1664], w_in[:, 1664:1792], w_in[:, 1792:1920],
        w_in[:, 2048:2176], w_in[:, 1920:2048], w_in[:, 2176:2304], w_in[:, 2304:2328]], axis=1))
    gcol = np.concatenate([_col(f(k)[0]) for k in ('ffn1_pre', 'ffn1_post', 'mix_pre', 'mix_post', 'xa_pre', 'xa_post', 'ffn2_pre', 'ffn2_post')]
                          + [_col(f('out_gain_a')[0]), _col(f('ck_b1')[0]), _col(f('cv_b1')[0])], axis=1)
    peT = np.ascontiguousarray(np.concatenate([f('ck_pe')[0].T, f('cv_pe')[0].T], axis=1))
    rowv = np.concatenate([f('gm_ln_g')[0].reshape(-1), f('gm_ln_b')[0].reshape(-1), f('out_gain_b')[0].reshape(-1),
                           f('mem_norm')[0].reshape(-1), f('gm_bs')[0].reshape(-1)])[None, :]
    shared = dict(
        wg1=f('ffn1_wg')[0], wu1=f('ffn1_wu')[0], wd1=f('ffn1_wd')[0], wg2=f('ffn2_wg')[0], wu2=f('ffn2_wu')[0], wd2=f('ffn2_wd')[0],
        w_in=w_in_r, w_out=f('w_out')[0], gm_ws=f('gm_ws')[0], ck_w1=f('ck_w1')[0], cv_w1=f('cv_w1')[0], ck_w2=f('ck_w2')[0], cv_w2=f('cv_w2')[0],
        xa_wq=f('xa_wq')[0], xa_wkv=f('xa_wkv')[0], xa_wo=f('xa_wo')[0], gcol=np.ascontiguousarray(gcol), peT=peT,
        rowv=np.ascontiguousarray(rowv), rel_bias=f('rel_bias'))
    consts = [_consts(r) for r in range(4)]
    in_maps = []
    for c in range(8):
        b, r = c // 4, c % 4
        shift = 128 * (3 - r)
        xc = np.zeros((SEQ, D), np.float32)
        xc[shift:] = x[b, :SEQ - shift]
        m = dict(shared)
        m.update(consts[r])
        m['x_ctx'] = xc
        m['mem'] = np.ascontiguousarray(mem[b])
        in_maps.append(m)
    nc = build(stage=STAGE, debug=DEBUG)
    res = run_bass_kernel_spmd(nc, in_maps, core_ids=list(range(8)))
    _last['res'] = res
    out = np.zeros((2, SEQ, D), np.float32)
    for c in range(8):
        b, r = c // 4, c % 4
        y = res.results[c]["y_own"].reshape(NOWN, 128, D)
        for i in range(NOWN):
            qt = 4 * i + r
            out[b, qt * 128:(qt + 1) * 128] = y[i]
    return out
```
